# Optimizing a Trainium2 kernel written in Bass

```python
import math
import jax, jax.numpy as jnp
from jax import lax
import numpy as np

D_MODEL = 1024
BATCH = 16
SEQ = 2048
DEPTH = 1

NSA_HEADS = 8
NSA_KV_GROUPS = 2
NSA_HEADS_PER_GROUP = NSA_HEADS // NSA_KV_GROUPS
HEAD_DIM = 64
NSA_Q_WIDTH = NSA_HEADS * HEAD_DIM
NSA_KV_WIDTH = NSA_KV_GROUPS * HEAD_DIM
CMP_BLOCK = 32
CMP_STRIDE = 16
CMP_HIDDEN = 2 * HEAD_DIM
SEL_BLOCK = 64
SEL_TOPN = 8
SEL_QBLOCK = 64
WINDOW = 256
WIN_QBLOCK = 128
ATTN_SCALE = HEAD_DIM ** -0.5

MLSTM_HEADS = 4
MLSTM_HEAD_DIM = 128
MLSTM_WIDTH = MLSTM_HEADS * MLSTM_HEAD_DIM
MLSTM_CHUNK = 128
CONV_WIDTH = 4

N_EXPERTS = 32
TOP_K = 4
D_EXPERT = D_MODEL
SWIGLU_LIMIT = 7.0
SWIGLU_ALPHA = 1.702
MOE_BLOCK = 256

RMS_EPS = 1e-5
LN_EPS = 1e-5

IN_SIZES = (NSA_Q_WIDTH, 6 * NSA_KV_WIDTH, 3 * NSA_HEADS, MLSTM_WIDTH, MLSTM_WIDTH, 2 * MLSTM_HEADS, 2 * D_MODEL)
IN_WIDTH = sum(IN_SIZES)
IN_SPLITS = [int(v) for v in np.cumsum(IN_SIZES)[:-1]]

kernel_name = 'hybrid_nsa_mlstm_moe_block'


def rms_norm(x, g):
    xf = x.astype(jnp.float32)
    y = xf * lax.rsqrt(jnp.mean(xf * xf, axis=-1, keepdims=True) + RMS_EPS)
    return (y * g.astype(jnp.float32)).astype(x.dtype)


def alibi_slopes(n_heads):
    return jnp.asarray(2.0 ** (-8.0 * np.arange(1, n_heads + 1) / n_heads), jnp.float32)


def masked_softmax(s, mask):
    s = jnp.where(mask, s, -jnp.inf)
    m = jnp.max(s, axis=-1, keepdims=True)
    m = jnp.where(jnp.isfinite(m), m, 0.0)
    p = jnp.where(mask, jnp.exp(s - m), 0.0)
    return p / jnp.maximum(jnp.sum(p, axis=-1, keepdims=True), 1e-30)


def compress_blocks(kv, pe, w1, w2):
    S = kv.shape[1]
    n_cmp = (S - CMP_BLOCK) // CMP_STRIDE + 1
    idx = np.arange(n_cmp)[:, None] * CMP_STRIDE + np.arange(CMP_BLOCK)[None, :]
    blocks = kv[:, idx] + pe[None, None, :, None, :]
    hid = jax.nn.gelu(jnp.einsum('bnlgd,lde->bnge', blocks, w1))
    return jnp.einsum('bnge,ed->bngd', hid, w2)


def compressed_branch(qg, kc, vc, slopes):
    S = qg.shape[1]
    n_cmp = kc.shape[1]
    t = np.arange(S)[:, None]
    start = np.arange(n_cmp)[None, :] * CMP_STRIDE
    mask = (start + CMP_BLOCK - 1) <= t
    dist = (t - start - (CMP_BLOCK - 1) / 2.0).astype(np.float32)
    s = jnp.einsum('bsghd,bngd->bghsn', qg, kc).astype(jnp.float32) * ATTN_SCALE
    s = s - slopes[None, :, :, None, None] * dist
    p = masked_softmax(s, mask)
    o = jnp.einsum('bghsn,bngd->bsghd', p.astype(vc.dtype), vc)
    return o, p


def selection_overlap(n_cmp, n_slc):
    cs = np.arange(n_cmp)[:, None] * CMP_STRIDE
    ss = np.arange(n_slc)[None, :] * SEL_BLOCK
    ov = np.clip(np.minimum(cs + CMP_BLOCK, ss + SEL_BLOCK) - np.maximum(cs, ss), 0, None)
    return jnp.asarray(ov / CMP_BLOCK, jnp.float32)


def selected_branch(qg, k_slc, v_slc, p_cmp, slopes):
    B, S, G, HPG, dh = qg.shape
    n_slc = S // SEL_BLOCK
    n_cmp = p_cmp.shape[-1]
    top_n = min(SEL_TOPN, n_slc)
    score = jnp.einsum('bghsn,nj->bgsj', p_cmp, selection_overlap(n_cmp, n_slc))
    t = np.arange(S)
    cur = t // SEL_BLOCK
    j = np.arange(n_slc)[None, :]
    forced = (j == cur[:, None]) | (j == 0)
    future = j > cur[:, None]
    score = jnp.where(forced, jnp.inf, jnp.where(future, -jnp.inf, score))
    _, idx = lax.top_k(score, top_n)
    valid = idx <= jnp.asarray(cur)[:, None]
    kb = k_slc.reshape(B, n_slc, SEL_BLOCK, G, dh).transpose(0, 3, 1, 2, 4)
    vb = v_slc.reshape(B, n_slc, SEL_BLOCK, G, dh).transpose(0, 3, 1, 2, 4)
    nq = S // SEL_QBLOCK

    def to_chunks(a, axis):
        shp = a.shape
        a = a.reshape(shp[:axis] + (nq, SEL_QBLOCK) + shp[axis + 1:])
        return jnp.moveaxis(a, axis, 0)

    q_c = to_chunks(qg, 1)
    idx_c = to_chunks(idx, 2)
    val_c = to_chunks(valid, 2)
    t_c = jnp.arange(S, dtype=jnp.int32).reshape(nq, SEL_QBLOCK)
    gather = jax.vmap(jax.vmap(lambda blocks, ix: blocks[ix]))
    offs = jnp.arange(SEL_BLOCK, dtype=jnp.int32)
    flat = (B, G, HPG, SEL_QBLOCK, top_n * SEL_BLOCK)

    def one_chunk(args):
        qc, ic, vc_, tc = args
        kg = gather(kb, ic)
        vg = gather(vb, ic)
        dist = tc[None, None, :, None, None] - (ic[..., None] * SEL_BLOCK + offs)
        mask = vc_[..., None] & (dist >= 0)
        s = jnp.einsum('bqghd,bgqnld->bghqnl', qc, kg).astype(jnp.float32) * ATTN_SCALE
        s = s - slopes[None, :, :, None, None, None] * dist[:, :, None].astype(jnp.float32)
        m6 = jnp.broadcast_to(mask[:, :, None], s.shape)
        p = masked_softmax(s.reshape(flat), m6.reshape(flat)).reshape(s.shape)
        return jnp.einsum('bghqnl,bgqnld->bqghd', p.astype(vg.dtype), vg)

    o = lax.map(one_chunk, (q_c, idx_c, val_c, t_c))
    return jnp.moveaxis(o, 0, 1).reshape(B, S, G, HPG, dh)


def window_branch(qg, k_win, v_win, slopes):
    B, S, G, HPG, dh = qg.shape
    nb = S // WIN_QBLOCK
    span = WINDOW + WIN_QBLOCK
    pad = ((0, 0), (WINDOW, 0), (0, 0), (0, 0))
    kp = jnp.pad(k_win, pad)
    vp = jnp.pad(v_win, pad)
    idx = np.arange(nb)[:, None] * WIN_QBLOCK + np.arange(span)[None, :]
    kb = kp[:, idx]
    vb = vp[:, idx]
    qb = qg.reshape(B, nb, WIN_QBLOCK, G, HPG, dh)
    qpos = np.arange(WIN_QBLOCK)[:, None]
    kpos = np.arange(span)[None, :]
    dist = qpos - kpos + WINDOW
    kabs = np.arange(nb)[:, None, None] * WIN_QBLOCK - WINDOW + kpos[None]
    mask = (dist >= 0) & (dist < WINDOW) & (kabs >= 0)
    s = jnp.einsum('biqghd,bikgd->bghiqk', qb, kb).astype(jnp.float32) * ATTN_SCALE
    s = s - slopes[None, :, :, None, None, None] * dist.astype(np.float32)
    p = masked_softmax(s, mask)
    o = jnp.einsum('bghiqk,bikgd->biqghd', p.astype(vb.dtype), vb)
    return o.reshape(B, S, G, HPG, dh)


def mlstm_branch(x_m, i_pre, f_pre, o_pre, conv_w, conv_b, wq, wk, wv, f_bias, norm_g):
    B, S, C = x_m.shape
    H, dh = MLSTM_HEADS, MLSTM_HEAD_DIM
    xc = lax.conv_general_dilated(x_m, conv_w[:, None, :], window_strides=(1,), padding=[(CONV_WIDTH - 1, 0)],
                                  dimension_numbers=('NWC', 'WIO', 'NWC'), feature_group_count=C)
    xc = jax.nn.silu(xc + conv_b)
    xch = xc.reshape(B, S, H, dh)
    xmh = x_m.reshape(B, S, H, dh)
    q = jnp.einsum('bshd,hde->bhse', xch, wq).astype(jnp.float32)
    k = jnp.einsum('bshd,hde->bhse', xch, wk).astype(jnp.float32) / math.sqrt(dh)
    v = jnp.einsum('bshd,hde->bhse', xmh, wv).astype(jnp.float32)
    ig = i_pre.astype(jnp.float32).transpose(0, 2, 1)
    lf = jax.nn.log_sigmoid((f_pre + f_bias).astype(jnp.float32)).transpose(0, 2, 1)
    L = MLSTM_CHUNK
    nc = S // L

    def chunks(a):
        a = a.reshape(a.shape[:2] + (nc, L) + a.shape[3:])
        return jnp.moveaxis(a, 2, 0)

    causal = np.tril(np.ones((L, L), dtype=bool))

    def step(carry, inp):
        Cm, nm, mm = carry
        qc, kc, vc, ic, fc = inp
        F = jnp.cumsum(fc, axis=-1)
        logD = jnp.where(causal, F[..., :, None] - F[..., None, :] + ic[..., None, :], -jnp.inf)
        inter = F + mm[..., None]
        m_t = jnp.maximum(inter, jnp.max(logD, axis=-1))
        Dm = jnp.exp(logD - m_t[..., None])
        wi = jnp.exp(inter - m_t)
        qk = jnp.einsum('bhtd,bhsd->bhts', qc, kc) * Dm
        num = wi[..., None] * jnp.einsum('bhtd,bhde->bhte', qc, Cm) + jnp.einsum('bhts,bhse->bhte', qk, vc)
        den = wi * jnp.einsum('bhtd,bhd->bht', qc, nm) + jnp.sum(qk, axis=-1)
        h = num / jnp.maximum(jnp.abs(den), jnp.exp(-m_t))[..., None]
        FL = F[..., -1]
        logw = FL[..., None] - F + ic
        m_new = jnp.maximum(FL + mm, jnp.max(logw, axis=-1))
        decay = jnp.exp(FL + mm - m_new)
        w = jnp.exp(logw - m_new[..., None])
        C_new = decay[..., None, None] * Cm + jnp.einsum('bhs,bhsd,bhse->bhde', w, kc, vc)
        n_new = decay[..., None] * nm + jnp.einsum('bhs,bhsd->bhd', w, kc)
        return (C_new, n_new, m_new), h

    init = (jnp.zeros((B, H, dh, dh), jnp.float32), jnp.zeros((B, H, dh), jnp.float32), jnp.zeros((B, H), jnp.float32))
    _, hs = lax.scan(step, init, (chunks(q), chunks(k), chunks(v), chunks(ig), chunks(lf)))
    h = jnp.moveaxis(hs, 0, 2).reshape(B, H, S, dh).transpose(0, 2, 1, 3)
    mu = jnp.mean(h, axis=-1, keepdims=True)
    var = jnp.mean(jnp.square(h - mu), axis=-1, keepdims=True)
    hn = ((h - mu) * lax.rsqrt(var + LN_EPS)).reshape(B, S, C) * norm_g.astype(jnp.float32)
    return (jax.nn.sigmoid(o_pre.astype(jnp.float32)) * hn).astype(x_m.dtype)


def moe_ffn(h, router_w, router_b, w_up, b_up, w_down, b_down):
    B, S, D = h.shape
    T = B * S
    R = T * TOP_K
    xt = h.reshape(T, D)
    logits = (xt @ router_w + router_b).astype(jnp.float32)
    top_v, top_e = lax.top_k(logits, TOP_K)
    wts = jax.nn.softmax(top_v, axis=-1)
    e_flat = top_e.reshape(R)
    onehot = jax.nn.one_hot(e_flat, N_EXPERTS, dtype=jnp.int32)
    rank = jnp.sum((jnp.cumsum(onehot, axis=0) - 1) * onehot, axis=-1)
    counts = jnp.sum(onehot, axis=0)
    padded = (counts + MOE_BLOCK - 1) // MOE_BLOCK * MOE_BLOCK
    pends = jnp.cumsum(padded)
    dest = (pends - padded)[e_flat] + rank
    n_blocks = (R + N_EXPERTS * (MOE_BLOCK - 1) + MOE_BLOCK - 1) // MOE_BLOCK
    P = n_blocks * MOE_BLOCK
    xpad = jnp.zeros((P, D), h.dtype).at[dest].set(jnp.repeat(xt, TOP_K, axis=0))
    block_start = jnp.arange(n_blocks, dtype=jnp.int32) * MOE_BLOCK
    block_e = jnp.minimum(jnp.searchsorted(pends, block_start, side='right'), N_EXPERTS - 1)

    def expert_block(args):
        xb, e = args
        gu = xb @ w_up[e] + b_up[e]
        g, lin = jnp.split(gu, 2, axis=-1)
        g = jnp.minimum(g, SWIGLU_LIMIT)
        lin = jnp.clip(lin, -SWIGLU_LIMIT, SWIGLU_LIMIT)
        a = g * jax.nn.sigmoid(SWIGLU_ALPHA * g) * (lin + 1.0)
        return a @ w_down[e] + b_down[e]

    ypad = lax.map(expert_block, (xpad.reshape(n_blocks, MOE_BLOCK, D), block_e)).reshape(P, D)
    y_rows = ypad[dest].reshape(T, TOP_K, D)
    out = jnp.einsum('tk,tkd->td', wts.astype(y_rows.dtype), y_rows)
    return out.reshape(B, S, D)


def hybrid_layer(x, c, ada_w, ada_b, norm1_g, w_in, b_in, cmp_pe_k, cmp_w1_k, cmp_w2_k, cmp_pe_v, cmp_w1_v, cmp_w2_v,
                 ml_conv_w, ml_conv_b, ml_wq, ml_wk, ml_wv, ml_f_bias, ml_norm_g, proj_a, proj_b, w_out, norm2_g,
                 router_w, router_b, exp_w_up, exp_b_up, exp_w_down, exp_b_down):
    B, S, D = x.shape
    G, HPG = NSA_KV_GROUPS, NSA_HEADS_PER_GROUP
    mod = jax.nn.silu(c) @ ada_w + ada_b
    shift1, scale1, gate1, shift2, scale2, gate2 = jnp.split(mod[:, None, :], 6, axis=-1)
    h = rms_norm(x, norm1_g) * (1.0 + scale1) + shift1
    proj = h @ w_in + b_in
    q, kv, g_nsa, x_m, o_pre, if_pre, g_merge = jnp.split(proj, IN_SPLITS, axis=-1)
    qg = q.reshape(B, S, G, HPG, HEAD_DIM)
    k_cmp, v_cmp, k_slc, v_slc, k_win, v_win = [a.reshape(B, S, G, HEAD_DIM) for a in jnp.split(kv, 6, axis=-1)]
    slopes = alibi_slopes(NSA_HEADS).reshape(G, HPG)
    kc = compress_blocks(k_cmp, cmp_pe_k, cmp_w1_k, cmp_w2_k)
    vc = compress_blocks(v_cmp, cmp_pe_v, cmp_w1_v, cmp_w2_v)
    o_cmp, p_cmp = compressed_branch(qg, kc, vc, slopes)
    o_slc = selected_branch(qg, k_slc, v_slc, p_cmp, slopes)
    o_win = window_branch(qg, k_win, v_win, slopes)
    bg = jax.nn.sigmoid(g_nsa).reshape(B, S, 3, G, HPG, 1)
    o_nsa = (bg[:, :, 0] * o_cmp + bg[:, :, 1] * o_slc + bg[:, :, 2] * o_win).reshape(B, S, NSA_Q_WIDTH)
    i_pre, f_pre = jnp.split(if_pre, 2, axis=-1)
    y_ml = mlstm_branch(x_m, i_pre, f_pre, o_pre, ml_conv_w, ml_conv_b, ml_wq, ml_wk, ml_wv, ml_f_bias, ml_norm_g)
    g_a, g_b = jnp.split(jax.nn.sigmoid(g_merge), 2, axis=-1)
    mixed = (g_a * (o_nsa @ proj_a) + g_b * (y_ml @ proj_b)) @ w_out
    x = x + gate1 * mixed
    h2 = rms_norm(x, norm2_g) * (1.0 + scale2) + shift2
    x = x + gate2 * moe_ffn(h2, router_w, router_b, exp_w_up, exp_b_up, exp_w_down, exp_b_down)
    return x


def setup_inputs(seed: int = 0) -> dict:
    key = jax.random.key(seed)
    keys = iter(jax.random.split(key, 40))

    def nrm(shape, scale):
        return jax.random.normal(next(keys), shape, jnp.float32) * scale

    L, D = DEPTH, D_MODEL
    return {
        'x': nrm((BATCH, SEQ, D), 1.0),
        'c': nrm((BATCH, D), 1.0),
        'ada_w': nrm((L, D, 6 * D), 0.5 * D ** -0.5),
        'ada_b': nrm((L, 6 * D), 0.02),
        'norm1_g': 1.0 + nrm((L, D), 0.05),
        'w_in': nrm((L, D, IN_WIDTH), D ** -0.5),
        'b_in': nrm((L, IN_WIDTH), 0.02),
        'cmp_pe_k': nrm((L, CMP_BLOCK, HEAD_DIM), 0.02),
        'cmp_w1_k': nrm((L, CMP_BLOCK, HEAD_DIM, CMP_HIDDEN), (CMP_BLOCK * HEAD_DIM) ** -0.5),
        'cmp_w2_k': nrm((L, CMP_HIDDEN, HEAD_DIM), CMP_HIDDEN ** -0.5),
        'cmp_pe_v': nrm((L, CMP_BLOCK, HEAD_DIM), 0.02),
        'cmp_w1_v': nrm((L, CMP_BLOCK, HEAD_DIM, CMP_HIDDEN), (CMP_BLOCK * HEAD_DIM) ** -0.5),
        'cmp_w2_v': nrm((L, CMP_HIDDEN, HEAD_DIM), CMP_HIDDEN ** -0.5),
        'ml_conv_w': nrm((L, CONV_WIDTH, MLSTM_WIDTH), CONV_WIDTH ** -0.5),
        'ml_conv_b': nrm((L, MLSTM_WIDTH), 0.02),
        'ml_wq': nrm((L, MLSTM_HEADS, MLSTM_HEAD_DIM, MLSTM_HEAD_DIM), MLSTM_HEAD_DIM ** -0.5),
        'ml_wk': nrm((L, MLSTM_HEADS, MLSTM_HEAD_DIM, MLSTM_HEAD_DIM), MLSTM_HEAD_DIM ** -0.5),
        'ml_wv': nrm((L, MLSTM_HEADS, MLSTM_HEAD_DIM, MLSTM_HEAD_DIM), MLSTM_HEAD_DIM ** -0.5),
        'ml_f_bias': jnp.linspace(3.0, 6.0, MLSTM_HEADS, dtype=jnp.float32)[None, :] + nrm((L, MLSTM_HEADS), 0.1),
        'ml_norm_g': 1.0 + nrm((L, MLSTM_WIDTH), 0.05),
        'proj_a': nrm((L, NSA_Q_WIDTH, D), NSA_Q_WIDTH ** -0.5),
        'proj_b': nrm((L, MLSTM_WIDTH, D), MLSTM_WIDTH ** -0.5),
        'w_out': nrm((L, D, D), D ** -0.5),
        'norm2_g': 1.0 + nrm((L, D), 0.05),
        'router_w': nrm((L, D, N_EXPERTS), D ** -0.5),
        'router_b': nrm((L, N_EXPERTS), 0.01),
        'exp_w_up': nrm((L, N_EXPERTS, D, 2 * D_EXPERT), D ** -0.5),
        'exp_b_up': nrm((L, N_EXPERTS, 2 * D_EXPERT), 0.02),
        'exp_w_down': nrm((L, N_EXPERTS, D_EXPERT, D), D_EXPERT ** -0.5),
        'exp_b_down': nrm((L, N_EXPERTS, D), 0.02),
        'final_g': 1.0 + nrm((D,), 0.05),
    }


def reference(x, c, ada_w, ada_b, norm1_g, w_in, b_in, cmp_pe_k, cmp_w1_k, cmp_w2_k, cmp_pe_v, cmp_w1_v, cmp_w2_v,
              ml_conv_w, ml_conv_b, ml_wq, ml_wk, ml_wv, ml_f_bias, ml_norm_g, proj_a, proj_b, w_out, norm2_g,
              router_w, router_b, exp_w_up, exp_b_up, exp_w_down, exp_b_down, final_g):
    for l in range(DEPTH):
        x = hybrid_layer(x, c, ada_w[l], ada_b[l], norm1_g[l], w_in[l], b_in[l],
                         cmp_pe_k[l], cmp_w1_k[l], cmp_w2_k[l], cmp_pe_v[l], cmp_w1_v[l], cmp_w2_v[l],
                         ml_conv_w[l], ml_conv_b[l], ml_wq[l], ml_wk[l], ml_wv[l], ml_f_bias[l], ml_norm_g[l],
                         proj_a[l], proj_b[l], w_out[l], norm2_g[l],
                         router_w[l], router_b[l], exp_w_up[l], exp_b_up[l], exp_w_down[l], exp_b_down[l])
    return rms_norm(x, final_g)
```

```python
import contextlib
import numpy as np
import ml_dtypes
import concourse.bass as bass
import concourse.mybir as mybir
from concourse.bass_utils import run_bass_kernel_spmd

F32 = mybir.dt.float32
BF16 = mybir.dt.bfloat16
AF = mybir.ActivationFunctionType
ALU = mybir.AluOpType
AX = mybir.AxisListType

NCORES = 8
D = 1024
S = 2048
NB = 2
NT = S // 128
KC = D // 128
NEG = -30000.0


class Buf:
    __slots__ = ("name", "last_w", "readers")

    def __init__(self, name):
        self.name = name
        self.last_w = None
        self.readers = {}


class Eng:
    def __init__(self, kb, name, h, is_pe=False):
        self.kb, self.name, self.h, self.is_pe = kb, name, h, is_pe
        self.sem = kb.new_sem("e_" + name)
        self.count = 0
        self.seen = {}

    def wait(self, tk):
        if tk is None:
            return
        sem, val = tk
        if sem is self.sem and self.is_pe:
            return
        if self.seen.get(sem, 0) >= val:
            return
        self.h.wait_ge(sem, val)
        self.seen[sem] = val


class DmaQ:
    def __init__(self, kb, name, eng, nsem=8):
        self.kb, self.eng = kb, eng
        self.sems = [kb.new_sem("d_%s%d" % (name, i)) for i in range(nsem)]
        self.vals = [0] * nsem
        self.n = 0


class KB:
    def __init__(self):
        self.nc = bass.Bass("TRN2", target_bir_lowering=False)
        self.root = contextlib.ExitStack()
        self._semn = 0
        self.in_names = []
        nc = self.nc
        self.pe = Eng(self, "pe", nc.tensor, is_pe=True)
        self.act = Eng(self, "act", nc.scalar)
        self.dve = Eng(self, "dve", nc.vector)
        self.pool = Eng(self, "pool", nc.gpsimd)
        self.sp = Eng(self, "sp", nc.sync)
        self.engs = [self.pe, self.act, self.dve, self.pool, self.sp]
        self.qs = DmaQ(self, "s", self.sp, 12)
        self.qg = DmaQ(self, "g", self.pool, 8)
        self.dqs = [self.qs, self.qg]

    def new_sem(self, name):
        self._semn += 1
        return self.root.enter_context(self.nc.semaphore("%s_%d" % (name, self._semn)))

    def dram_in(self, name, shape, dtype=F32):
        self.in_names.append(name)
        return self.nc.dram_tensor(name, list(shape), dtype, kind="ExternalInput").ap()

    def dram_out(self, name, shape, dtype=F32):
        return self.nc.dram_tensor(name, list(shape), dtype, kind="ExternalOutput").ap()

    def dram_tmp(self, name, shape, dtype=F32):
        return self.nc.dram_tensor(name, list(shape), dtype, kind="Internal").ap()

    def sb(self, stack, name, shape, dtype):
        self._semn += 1
        return stack.enter_context(self.nc.sbuf_tensor("sb%d_%s" % (self._semn, name), list(shape), dtype))

    def ps(self, stack, name, shape, dtype):
        self._semn += 1
        return stack.enter_context(self.nc.psum_tensor("pp%d_%s" % (self._semn, name), list(shape), dtype))

    def _deps(self, eng, reads, writes):
        for b in reads:
            eng.wait(b.last_w)
        for b in writes:
            eng.wait(b.last_w)
            for sem, val in b.readers.items():
                eng.wait((sem, val))

    def _commit(self, tk, reads, writes):
        for b in reads:
            sem, val = tk
            if b.readers.get(sem, 0) < val:
                b.readers[sem] = val
        for b in writes:
            b.last_w = tk
            b.readers = {}

    def op(self, eng, fn, reads=(), writes=()):
        self._deps(eng, reads, writes)
        ins = fn()
        eng.count += 1
        ins.then_inc(eng.sem, 1)
        tk = (eng.sem, eng.count)
        self._commit(tk, reads, writes)
        return tk

    def dma(self, q, out, in_, reads=(), writes=()):
        eng = q.eng
        self._deps(eng, reads, writes)
        slot = q.n % len(q.sems)
        q.n += 1
        sem = q.sems[slot]
        if q.vals[slot]:
            eng.wait((sem, q.vals[slot]))
        ins = eng.h.dma_start(out=out, in_=in_)
        ins.then_inc(sem, 16)
        q.vals[slot] += 16
        tk = (sem, q.vals[slot])
        self._commit(tk, reads, writes)
        return tk

    def barrier(self):
        tks = [(e.sem, e.count) for e in self.engs if e.count]
        for q in self.dqs:
            tks += [(s, v) for s, v in zip(q.sems, q.vals) if v]
        for e in self.engs:
            for tk in tks:
                if tk[0] is e.sem:
                    continue
                e.wait(tk)

    def mm(self, out, lhsT, rhs, start, stop, reads=(), writes=()):
        nc = self.nc
        return self.op(self.pe, lambda: nc.tensor.matmul(out, lhsT, rhs, start=start, stop=stop,
                                                         skip_group_check=True), reads, writes)

    def tr(self, out, in_, ident, reads=(), writes=()):
        nc = self.nc
        return self.op(self.pe, lambda: nc.tensor.transpose(out, in_, ident), reads, writes)

    def actf(self, out, in_, func, bias=None, scale=None, accum_out=None, reads=(), writes=()):
        nc = self.nc
        kw = {}
        if bias is not None:
            kw["bias"] = bias
        if scale is not None:
            kw["scale"] = scale
        if accum_out is not None:
            kw["accum_out"] = accum_out
        return self.op(self.act, lambda: nc.scalar.activation(out=out, in_=in_, func=func, **kw), reads, writes)

    def ts(self, eng, out, in0, s1, s2, op0, op1=None, reads=(), writes=()):
        kw = {}
        if op1 is not None:
            kw["op1"] = op1
        return self.op(eng, lambda: eng.h.tensor_scalar(out, in0, s1, s2, op0, **kw), reads, writes)

    def tt(self, eng, out, in0, in1, op, reads=(), writes=()):
        return self.op(eng, lambda: eng.h.tensor_tensor(out, in0, in1, op), reads, writes)

    def stt(self, eng, out, in0, scalar, in1, op0, op1, reads=(), writes=()):
        return self.op(eng, lambda: eng.h.scalar_tensor_tensor(out, in0, scalar, in1, op0, op1), reads, writes)

    def cp(self, eng, out, in_, reads=(), writes=()):
        return self.op(eng, lambda: eng.h.tensor_copy(out, in_), reads, writes)

    def memset(self, eng, ap, val, writes=()):
        return self.op(eng, lambda: eng.h.memset(ap, val), (), writes)


def host_consts():
    c = {}
    c["ident_f"] = np.eye(128, dtype=np.float32)
    slopes = (2.0 ** (-8.0 * np.arange(1, 9) / 8)).astype(np.float64)
    n = np.arange(128)[:, None, None]
    qt = np.arange(NT)[None, :, None]
    q = np.arange(128)[None, None, :]
    cm = ((16 * n + 31) <= (128 * qt + q)) & (n < 127)
    c["cmask"] = cm.astype(np.float32)
    nn = np.arange(128)[:, None, None]
    hh = np.arange(8)[None, :, None]
    qq = np.arange(NT)[None, None, :]
    cb = slopes[hh] * (16 * nn + 15.5 - 128 * qq)
    cb = np.where((nn <= 8 * qq + 6) & (nn < 127), cb, NEG)
    c["cbias"] = cb.astype(np.float32)
    cs = np.arange(127)[:, None] * 16
    ss = np.arange(32)[None, :] * 64
    ov = np.clip(np.minimum(cs + 32, ss + 64) - np.maximum(cs, ss), 0, None) / 32.0
    ovp = np.zeros((128, 32), np.float32)
    ovp[:127] = ov
    c["ov"] = ovp
    qv = np.arange(128)[:, None, None]
    qtv = np.arange(NT)[None, :, None]
    jv = np.arange(32)[None, None, :]
    cur = 2 * qtv + (qv >= 64)
    forced = (jv == cur) | (jv == 0)
    fut = jv > cur
    c["fadj"] = np.where(forced, 1e4, np.where(fut, -1e4, 0.0)).astype(np.float32)
    c["notfut"] = (~fut).astype(np.float32)
    ex = np.zeros((128, NT, 128), np.float32)
    for kt in range(NT):
        ex[2 * kt, kt, 0:64] = 1.0
        ex[2 * kt + 1, kt, 64:128] = 1.0
    c["expand"] = ex
    p = np.arange(128)[:, None]
    qc = np.arange(128)[None, :]
    c["trineg"] = np.where(p > qc, NEG, 0.0).astype(np.float32)
    c["trineg2"] = np.where(p <= qc, NEG, 0.0).astype(np.float32)
    pv = np.arange(128)[:, None, None]
    dl = np.arange(16)[None, None, :]
    c["alibi"] = (slopes[hh] * (pv - 128 * dl)).astype(np.float32)
    c["tri_u"] = (p <= qc).astype(np.float32)
    bf = ml_dtypes.bfloat16
    al = np.zeros((128, 2, 16, 128), np.float32)
    hind = np.zeros((128, 4, 128), np.float32)
    pp = np.arange(128)[None, :]
    dd = np.arange(16)[:, None]
    for g in range(2):
        for h4 in range(4):
            val = (slopes[4 * g + h4] * (pp - 128 * dd)).astype(np.float32)
            hi = val.astype(bf).astype(np.float32)
            lo = (val - hi).astype(bf).astype(np.float32)
            al[2 * h4, g] = hi
            al[2 * h4 + 1, g] = lo
    for h4 in range(4):
        hind[2 * h4:2 * h4 + 2, h4, :] = 1.0
    c["AL"] = al
    c["hind"] = hind
    return c


FM_Q, FM_KC, FM_VC, FM_KS0, FM_KS1, FM_KW0, FM_KW1, FM_XM, FM_MG = 0, 4, 5, 6, 7, 8, 9, 10, 14
N_FM = 30
N_FM1 = 14
TM_A = 288
TM_W = 800


def fm_cols():
    cols = []
    for c in range(4):
        cols.append(np.arange(c * 128, (c + 1) * 128))
    kv0 = 512
    cols.append(kv0 + np.arange(0, 128))
    cols.append(kv0 + np.arange(128, 256))
    for base in (256, 512):
        for g in range(2):
            a = kv0 + base + g * 64 + np.arange(64)
            cols.append(np.concatenate([a, a]))
    xm0 = 512 + 768 + 24
    for c in range(4):
        cols.append(xm0 + np.arange(c * 128, (c + 1) * 128))
    mg0 = xm0 + 512 + 512 + 8
    for c in range(16):
        cols.append(mg0 + np.arange(c * 128, (c + 1) * 128))
    return np.stack(cols)


def tm_cols():
    kv0 = 512
    xm0 = 512 + 768 + 24
    return np.concatenate([kv0 + 384 + np.arange(128), kv0 + 640 + np.arange(128),
                           512 + 768 + np.arange(24), xm0 + 1024 + np.arange(8),
                           xm0 + 512 + np.arange(512)])


def colT(v, n):
    return np.ascontiguousarray(v.reshape(n, 128).T)


def build(stage=99, taps=()):
    kb = KB()
    nc = kb.nc
    root = kb.root
    pe, act, dve, pool, sp = kb.pe, kb.act, kb.dve, kb.pool, kb.sp
    qs, qg = kb.qs, kb.qg
    taps = set(taps)
    tap_out = {}

    x_d = kb.dram_in("x", [NB, S, D])
    cT_d = kb.dram_in("cT", [128, KC, NB])
    adaw_d = kb.dram_in("ada_w", [D, 6 * D])
    adabT_d = kb.dram_in("ada_bT", [128, 48])
    adabrow_d = kb.dram_in("ada_brow", [128, 2 * D])
    n1gT_d = kb.dram_in("n1gT", [128, KC])
    n2gT_d = kb.dram_in("n2gT", [128, KC])
    wfm_d = kb.dram_in("w_fm", [D, N_FM * 128])
    bfmT_d = kb.dram_in("b_fmT", [128, N_FM])
    wtm_d = kb.dram_in("w_tm", [D, TM_W])
    btm_d = kb.dram_in("b_tm", [128, TM_W])
    identf_d = kb.dram_in("ident_f", [128, 128])
    out_d = kb.dram_out("out", [NB, S, D]) if stage >= 6 else None

    def tap(name, ap_sb, shape, dtype=F32, reads=()):
        if name not in taps:
            return
        d = kb.dram_out("tap_" + name, shape, dtype)
        tap_out[name] = d
        kb.dma(qs, d, ap_sb, reads=reads)

    ident_f = kb.sb(root, "ident_f", [128, 128], F32)
    ident_b = kb.sb(root, "ident_b", [128, 128], BF16)
    ones_r = kb.sb(root, "ones_r", [128, 128], BF16)
    ones_rf = kb.sb(root, "ones_rf", [128, 128], F32)
    modT = kb.sb(root, "modT", [128, 48, NB], F32)
    s1T = kb.sb(root, "s1T", [128, KC, NB], F32)
    s2T = kb.sb(root, "s2T", [128, KC, NB], F32)
    gate_d = kb.dram_tmp("gate_scr", [NB, 2, 128, D])
    x1_d = (kb.dram_out if "x1" in taps else kb.dram_tmp)("x1_scr", [NB, S, D])
    h2T_d = (kb.dram_out if "h2T" in taps else kb.dram_tmp)("h2T_scr", [NB, 128, KC, S], BF16)
    B_gd, B_x1d, B_h2d = Buf("gate_d"), Buf("x1_d"), Buf("h2T_d")
    B_const = Buf("const")
    B_mod = Buf("mod")

    psA = [kb.ps(root, "psA%d" % i, [128, 512], F32) for i in range(6)]
    psT = [kb.ps(root, "psT%d" % i, [128, 1024], BF16) for i in range(2)]
    B_psA = [Buf("psA%d" % i) for i in range(6)]
    B_psT = [Buf("psT%d" % i) for i in range(2)]

    kb.dma(qs, ident_f[:], identf_d, writes=[B_const])
    kb.cp(dve, ident_b[:], ident_f[:], reads=[B_const], writes=[B_const])
    kb.memset(dve, ones_r[:], 0.0, writes=[B_const])
    kb.memset(dve, ones_r[0:1, :], 1.0, writes=[B_const])
    kb.memset(dve, ones_rf[:], 0.0, writes=[B_const])
    kb.memset(dve, ones_rf[0:1, :], 1.0, writes=[B_const])

    with contextlib.ExitStack() as st:
        cT = kb.sb(st, "cT", [128, KC, NB], F32)
        scT = kb.sb(st, "scT", [128, KC, NB], F32)
        scbc = kb.sb(st, "scbc", [128, KC, NB, 128], F32)
        adabT = kb.sb(st, "adabT", [128, 48], F32)
        adabrow = kb.sb(st, "adabrow", [128, 2 * D], F32)
        n1gT = kb.sb(st, "n1gT", [128, KC], F32)
        n2gT = kb.sb(st, "n2gT", [128, KC], F32)
        awb = [kb.sb(st, "awb%d" % i, [128, KC, D], F32) for i in range(2)]
        gstage = [kb.sb(st, "gstage%d" % i, [128, 512], F32) for i in range(2)]
        B_gs = [Buf("gs0"), Buf("gs1")]
        B_aw = [Buf("aw0"), Buf("aw1")]
        B_c = Buf("c")
        kb.dma(qs, cT[:], cT_d, writes=[B_c])
        kb.dma(qs, adabT[:], adabT_d, writes=[B_c])
        kb.dma(qs, adabrow[:], adabrow_d, writes=[B_c])
        kb.dma(qs, n1gT[:], n1gT_d, writes=[B_c])
        kb.dma(qs, n2gT[:], n2gT_d, writes=[B_c])
        kb.actf(scT[:], cT[:], AF.Silu, reads=[B_c], writes=[B_c])
        for b in range(NB):
            for k in range(KC):
                kb.cp(dve, scbc[:, k, b, :], scT[:, k, b:b + 1].to_broadcast([128, 128]), reads=[B_c], writes=[B_c])
        adaw_v = adaw_d.rearrange("(k p) n -> p k n", p=128)
        pm = psA[0]
        first = True
        for grp in range(6):
            wb = awb[grp % 2]
            kb.dma(qs, wb[:], adaw_v[:, :, grp * D:(grp + 1) * D], writes=[B_aw[grp % 2]])
            for j in range(8):
                jj = grp * 8 + j
                for k in range(KC):
                    kb.mm(pm[:, jj * NB:(jj + 1) * NB], wb[:, k, j * 128:(j + 1) * 128], scT[:, k, :],
                          start=(k == 0), stop=(k == KC - 1),
                          reads=[B_aw[grp % 2], B_c], writes=[B_psA[0]])
            if grp in (2, 5):
                gi = 0 if grp == 2 else 1
                for b in range(NB):
                    for hf in range(2):
                        pb = psA[1 + hf]
                        for k in range(KC):
                            kb.mm(pb[:], scbc[:, k, b, :], wb[:, k, hf * 512:(hf + 1) * 512],
                                  start=(k == 0), stop=False, reads=[B_aw[grp % 2], B_c], writes=[B_psA[1 + hf]])
                        kb.mm(pb[:], ones_rf[:], adabrow[:, gi * D + hf * 512: gi * D + (hf + 1) * 512],
                              start=False, stop=True, reads=[B_c, B_const], writes=[B_psA[1 + hf]])
                        kb.cp(dve, gstage[hf][:], pb[:], reads=[B_psA[1 + hf]], writes=[B_gs[hf]])
                        kb.dma(qs, gate_d[b, gi, :, hf * 512:(hf + 1) * 512], gstage[hf][:], reads=[B_gs[hf]], writes=[B_gd])
        for b in range(NB):
            pv = pm[:, 0:48 * NB].rearrange("p (j b) -> p j b", b=NB)
            kb.tt(dve, modT[:, :, b], pv[:, :, b], adabT[:], ALU.add, reads=[B_psA[0], B_c], writes=[B_mod])
        for b in range(NB):
            kb.stt(dve, s1T[:, :, b], modT[:, 8:16, b], 1.0, n1gT[:], ALU.add, ALU.mult, reads=[B_mod, B_c], writes=[B_mod])
            kb.stt(dve, s2T[:, :, b], modT[:, 32:40, b], 1.0, n2gT[:], ALU.add, ALU.mult, reads=[B_mod, B_c], writes=[B_mod])
        tap("modT", modT[:], [128, 48, NB], reads=[B_mod])
        kb.barrier()

    fbias_d = kb.dram_in("fbias_bc", [128, 4])
    RMS_EPS = 1e-5
    B_hT = Buf("hT")

    evac_rr = [0]

    def evac(out, in_, bias, mul=None, reads=(), writes=(), force_dve=False):
        evac_rr[0] += 1
        if force_dve or evac_rr[0] % 2 == 0:
            if mul is None:
                kb.ts(dve, out, in_, bias, None, ALU.add, reads=reads, writes=writes)
            else:
                kb.ts(dve, out, in_, bias, mul, ALU.add, ALU.mult, reads=reads, writes=writes)
        else:
            if mul is None:
                kb.actf(out, in_, AF.Identity, bias=bias, reads=reads, writes=writes)
            else:
                kb.ts(dve, out, in_, bias, mul, ALU.add, ALU.mult, reads=reads, writes=writes)

    cdram = {}
    for nm, shp in (("cmask", [128, NT, 128]), ("cbias", [128, 8, NT]), ("ov", [128, 32]),
                    ("fadj", [128, NT, 32]), ("notfut", [128, NT, 32]), ("expand", [128, NT, 128]),
                    ("trineg", [128, 128]), ("trineg2", [128, 128]), ("alibi", [128, 8, 16]),
                    ("tri_u", [128, 128]), ("AL", [128, 2, 16, 128]), ("hind", [128, 4, 128])):
        cdram[nm] = kb.dram_in(nm, shp)
    w1k_d = kb.dram_in("cmp_w1_k", [32, 64, 128])
    w1v_d = kb.dram_in("cmp_w1_v", [32, 64, 128])
    w2k_d = kb.dram_in("cmp_w2_k", [128, 64])
    w2v_d = kb.dram_in("cmp_w2_v", [128, 64])
    pek_d = kb.dram_in("peT_k", [128, 32])
    pev_d = kb.dram_in("peT_v", [128, 32])

    def nsa_phase(b, qT, kcT, vcT, ksT, kwT, vaug_s, vaug_w, sig_nsa, onsaT, B_onsaT, B_in):
        with contextlib.ExitStack() as st:
            B_c = Buf("nsaconst")
            cmask = kb.sb(st, "cmask", [128, NT, 128], BF16)
            cbias = kb.sb(st, "cbias", [128, 8, NT], F32)
            ov = kb.sb(st, "ov", [128, 32], F32)
            fadj = kb.sb(st, "fadj", [128, NT, 32], F32)
            notfut = kb.sb(st, "notfut", [128, NT, 32], F32)
            expand = kb.sb(st, "expand", [128, NT, 128], BF16)
            trineg = kb.sb(st, "trineg", [128, 128], BF16)
            trineg2 = kb.sb(st, "trineg2", [128, 128], BF16)
            AL = kb.sb(st, "AL", [128, 2, 16, 128], BF16)
            hind = kb.sb(st, "hind", [128, 4, 128], BF16)
            ones_f = kb.sb(st, "ones_f", [128, 128], F32)
            kb.dma(qg, AL[:], cdram["AL"], writes=[B_c])
            kb.dma(qg, hind[:], cdram["hind"], writes=[B_c])
            for t_, nm in ((cbias, "cbias"), (ov, "ov"), (fadj, "fadj"), (notfut, "notfut")):
                kb.dma(qs, t_[:], cdram[nm], writes=[B_c])
            for t_, nm in ((cmask, "cmask"), (expand, "expand"), (trineg, "trineg"), (trineg2, "trineg2")):
                kb.dma(qg, t_[:], cdram[nm], writes=[B_c])
            kb.memset(dve, ones_f[:], 1.0, writes=[B_c])
            kcc = [[kb.sb(st, "kcc%d%d" % (g, v), [128, 128], BF16) for v in range(2)] for g in range(2)]
            vcc = [kb.sb(st, "vcc%d" % g, [128, 64], BF16) for g in range(2)]
            st_outer = st
            st = contextlib.ExitStack()
            w1 = [kb.sb(st, "w1_%d" % i, [128, 32, 128], BF16) for i in range(2)]
            w2kd = kb.sb(st, "w2kd", [128, 128], BF16)
            w2v = kb.sb(st, "w2v", [128, 64], BF16)
            peT = [kb.sb(st, "peT%d" % i, [128, 32], BF16) for i in range(2)]
            for i, wd in enumerate((w1k_d, w1v_d)):
                v_ = wd.rearrange("l d e -> d l e")
                kb.dma(qg, w1[i][0:64, :, :], v_, writes=[B_c])
                kb.dma(qg, w1[i][64:128, :, :], v_, writes=[B_c])
            kb.dma(qg, w2kd[:, 0:64], w2k_d, writes=[B_c])
            kb.dma(qg, w2kd[:, 64:128], w2k_d, writes=[B_c])
            kb.dma(qg, w2v[:], w2v_d, writes=[B_c])
            kb.dma(qg, peT[0][:], pek_d, writes=[B_c])
            kb.dma(qg, peT[1][:], pev_d, writes=[B_c])
            cst = kb.sb(st, "cst", [128, 2], F32)
            u_ = kb.sb(st, "cu", [128, 128], F32)
            t_ = kb.sb(st, "ct", [128, 128], F32)
            ha = kb.sb(st, "cha", [128, 128], BF16)
            B_cc, B_cw = Buf("kcc"), Buf("cwork")
            for g in range(2):
                for v in range(2):
                    kb.memset(pool, kcc[g][v][:], 0.0, writes=[B_cc])
                kb.memset(pool, vcc[g][:], 0.0, writes=[B_cc])
            for i in range(2):
                for l in range(32):
                    kb.mm(psA[2][:, i:i + 1], w1[i][:, l, :], peT[i][:, l:l + 1], start=(l == 0), stop=(l == 31),
                          reads=[B_c], writes=[B_psA[2]])
            kb.cp(dve, cst[:], psA[2][:, 0:2], reads=[B_psA[2]], writes=[B_cw])
            for g in range(2):
                for i in range(2):
                    src = (kcT if i == 0 else vcT)[g]
                    sv = src[:, :].rearrange("p (n s) -> p n s", s=16)
                    ph = psA[i]
                    for l in range(32):
                        rhs = sv[:, 0:127, l] if l < 16 else sv[:, 1:128, l - 16]
                        kb.mm(ph[:, 0:127], w1[i][:, l, :], rhs, start=(l == 0), stop=(l == 31),
                              reads=[B_c] + B_in, writes=[B_psA[i]])
                    kb.ts(dve, u_[:, 0:127], ph[:, 0:127], cst[:, i:i + 1], None, ALU.add, reads=[B_psA[i], B_cw], writes=[B_cw])
                    kb.tt(dve, t_[:, 0:127], u_[:, 0:127], u_[:, 0:127], ALU.mult, reads=[B_cw], writes=[B_cw])
                    kb.ts(dve, t_[:, 0:127], t_[:, 0:127], 0.044715, 1.0, ALU.mult, ALU.add, reads=[B_cw], writes=[B_cw])
                    kb.tt(dve, t_[:, 0:127], t_[:, 0:127], u_[:, 0:127], ALU.mult, reads=[B_cw], writes=[B_cw])
                    kb.actf(t_[:, 0:127], t_[:, 0:127], AF.Sigmoid, scale=1.5957691216057308, reads=[B_cw], writes=[B_cw])
                    kb.tt(dve, ha[:, 0:127], t_[:, 0:127], u_[:, 0:127], ALU.mult, reads=[B_cw], writes=[B_cw])
                    if i == 0:
                        kb.mm(psA[3][:, 0:127], w2kd[:], ha[:, 0:127], start=True, stop=True, reads=[B_c, B_cw], writes=[B_psA[3]])
                        kb.cp(dve, kcc[g][0][0:64, 0:127], psA[3][0:64, 0:127], reads=[B_psA[3]], writes=[B_cc])
                        kb.cp(dve, kcc[g][1][64:128, 0:127], psA[3][64:128, 0:127], reads=[B_psA[3]], writes=[B_cc])
                    else:
                        kb.mm(psA[3][0:127, 0:64], ha[:, 0:127], w2v[:], start=True, stop=True, reads=[B_c, B_cw], writes=[B_psA[3]])
                        kb.cp(dve, vcc[g][0:127, :], psA[3][0:127, 0:64], reads=[B_psA[3]], writes=[B_cc])
            tap("kcc00", kcc[0][0][:], [128, 128], BF16, reads=[B_cc])
            tap("vcc1", vcc[1][:], [128, 64], BF16, reads=[B_cc])
            kb.barrier()
            st.close()
            st = st_outer

            e_sb = kb.sb(st, "e_sb", [128, 512], F32)
            em = kb.sb(st, "em", [128, 512], F32)
            rs = kb.sb(st, "rs", [128, 512], F32)
            p_f = kb.sb(st, "p_f", [128, 512], F32)
            p_b = kb.sb(st, "p_b", [128, 512], BF16)
            sadj = kb.sb(st, "sadj", [128, 32], F32)
            top8 = kb.sb(st, "top8", [128, 8], F32)
            sel = kb.sb(st, "sel", [128, 32], F32)
            nsel = kb.sb(st, "nsel", [128, 32], BF16)
            nselT = [kb.sb(st, "nselT%d" % i, [128, 128], BF16) for i in range(2)]
            pT = [kb.sb(st, "pT%d" % i, [128, 512], BF16) for i in range(3)]
            acc = [kb.sb(st, "acc%d" % i, [128, 512], F32) for i in range(2)]
            gsc = kb.sb(st, "gsc", [128, 4], F32)
            onb = kb.sb(st, "onb", [128, 512], BF16)
            B_e, B_p, B_sel, B_gsc, B_onb = Buf("e"), Buf("p"), Buf("sel"), Buf("gsc"), Buf("onb")
            B_nselT = [Buf("nselT0"), Buf("nselT1")]
            B_pT = [Buf("pT0"), Buf("pT1"), Buf("pT2")]
            B_acc = [Buf("acc0"), Buf("acc1")]
            for i in range(2):
                kb.memset(pool, nselT[i][:], 0.0, writes=[B_nselT[i]])
            kb.memset(pool, sel[:], 0.0, writes=[B_sel])
            sel_dbg = None
            if "sel" in taps:
                sel_dbg = kb.sb(st, "sel_dbg", [128, NT, 2, 32], F32)
            score_dbg = None
            if "score" in taps:
                score_dbg = kb.sb(st, "score_dbg", [128, NT, 2, 32], F32)
            npt = [0]
            nps = [0]

            def attn_branch(qt, g, kts, kT2, vaug, br, ac, Bac, nsT, BnsT, po, Bpo):
                for idx, (kt, mk) in enumerate(kts):
                    si = nps[0] % 2
                    nps[0] += 1
                    ps_ = psA[si]
                    Bps = B_psA[si]
                    ps4 = ps_[:].rearrange("p (h q) -> p h q", h=4)
                    started = False
                    if nsT is not None:
                        kb.mm(ps4, expand[:, kt, :], nsT[:, :].unsqueeze(1).to_broadcast([128, 4, 128]),
                              start=True, stop=False, reads=[B_c, BnsT], writes=[Bps])
                        started = True
                    if mk is not None:
                        kb.mm(ps4, ident_b[:], mk[:, :].unsqueeze(1).to_broadcast([128, 4, 128]),
                              start=not started, stop=False, reads=[B_c, B_const], writes=[Bps])
                        started = True
                    kb.mm(ps4, AL[:, g, qt - kt, :], hind[:], start=not started, stop=False, reads=[B_c], writes=[Bps])
                    started = True
                    for hh in range(4):
                        h = 4 * g + hh
                        kb.mm(ps_[:, hh * 128:(hh + 1) * 128], kT2[g][h % 2][:, kt * 128:(kt + 1) * 128],
                              qT[:, h // 2, qt * 128:(qt + 1) * 128], start=not started, stop=(hh == 3),
                              reads=B_in, writes=[Bps])
                        started = True
                    pi = npt[0] % 3
                    npt[0] += 1
                    kb.actf(pT[pi][:], ps_[:], AF.Exp, reads=[Bps], writes=[B_pT[pi]])
                    for hh in range(4):
                        kb.mm(po[:, hh * 65:(hh + 1) * 65], pT[pi][:, hh * 128:(hh + 1) * 128], vaug[:, kt, g, :],
                              start=(idx == 0 and hh == 0), stop=(idx == len(kts) - 1 and hh == 3),
                              reads=[B_pT[pi]] + B_in, writes=[Bpo])
                po3 = po[:, 0:260].rearrange("p (h c) -> p h c", c=65)
                kb.op(dve, lambda: nc.vector.reciprocal(gsc[:], po3[:, :, 64]), reads=[Bpo], writes=[B_gsc])
                kb.tt(dve, gsc[:], gsc[:], sig_nsa[:, qt, br * 8 + g * 4: br * 8 + g * 4 + 4], ALU.mult,
                      reads=[B_gsc] + B_in, writes=[B_gsc])
                for hh in range(4):
                    h = 4 * g + hh
                    kb.stt(dve, ac[:, h * 64:(h + 1) * 64], po[:, hh * 65: hh * 65 + 64], gsc[:, hh:hh + 1],
                           ac[:, h * 64:(h + 1) * 64], ALU.mult, ALU.add, reads=[Bpo, B_gsc], writes=[Bac])

            for qt in range(NT):
                ac = acc[qt % 2]
                Bac = B_acc[qt % 2]
                qs_ = slice(qt * 128, (qt + 1) * 128)
                for g in range(2):
                    si = nps[0] % 2
                    nps[0] += 1
                    ps_ = psA[si]
                    Bps = B_psA[si]
                    for hh in range(4):
                        h = 4 * g + hh
                        kb.mm(ps_[0:127, hh * 128:(hh + 1) * 128], kcc[g][h % 2][:, 0:127], qT[:, h // 2, qs_],
                              start=(hh == 0), stop=(hh == 3), reads=[B_cc] + B_in, writes=[Bps])
                    for hh in range(4):
                        h = 4 * g + hh
                        kb.actf(e_sb[0:127, hh * 128:(hh + 1) * 128], ps_[0:127, hh * 128:(hh + 1) * 128], AF.Exp,
                                bias=cbias[0:127, h, qt:qt + 1], reads=[Bps, B_c], writes=[B_e])
                    kb.tt(dve, em[0:127, :].rearrange("p (h q) -> p h q", h=4),
                          e_sb[0:127, :].rearrange("p (h q) -> p h q", h=4),
                          cmask[0:127, qt, :].unsqueeze(1).to_broadcast([127, 4, 128]), ALU.mult,
                          reads=[B_e, B_c], writes=[B_e])
                    kb.mm(psA[2][0:127, :], ones_f[0:127, 0:127], em[0:127, :], start=True, stop=True,
                          reads=[B_e, B_c], writes=[B_psA[2]])
                    kb.ts(dve, rs[0:127, :], psA[2][0:127, :], 1e-30, None, ALU.max, reads=[B_psA[2]], writes=[B_p])
                    kb.op(dve, lambda: nc.vector.reciprocal(rs[0:127, :], rs[0:127, :]), reads=[B_p], writes=[B_p])
                    kb.tt(dve, p_f[0:127, :], em[0:127, :], rs[0:127, :], ALU.mult, reads=[B_e, B_p], writes=[B_p])
                    kb.cp(pool, p_b[0:127, :], p_f[0:127, :], reads=[B_p], writes=[B_p])
                    pc = psA[3]
                    for hh in range(4):
                        kb.mm(pc[:, 256:288], p_f[0:127, hh * 128:(hh + 1) * 128], ov[0:127, :], start=(hh == 0), stop=False,
                              reads=[B_p, B_c], writes=[B_psA[3]])
                    for hh in range(4):
                        kb.mm(pc[:, hh * 64:(hh + 1) * 64], p_b[0:127, hh * 128:(hh + 1) * 128], vcc[g][0:127, :],
                              start=False, stop=(hh == 3), reads=[B_p, B_cc], writes=[B_psA[3]])
                    for hh in range(4):
                        h = 4 * g + hh
                        kb.ts(dve, ac[:, h * 64:(h + 1) * 64], pc[:, hh * 64:(hh + 1) * 64],
                              sig_nsa[:, qt, g * 4 + hh: g * 4 + hh + 1], None, ALU.mult,
                              reads=[B_psA[3]] + B_in, writes=[Bac])
                    kb.tt(dve, sadj[:], pc[:, 256:288], fadj[:, qt, :], ALU.add, reads=[B_psA[3], B_c], writes=[B_sel])
                    if score_dbg is not None:
                        kb.cp(dve, score_dbg[:, qt, g, :], pc[:, 256:288], reads=[B_psA[3]], writes=[B_sel])
                    kb.op(dve, lambda: nc.vector.max(out=top8[:], in_=sadj[:]), reads=[B_sel], writes=[B_sel])
                    kb.stt(dve, sel[:], sadj[:], top8[:, 7:8], notfut[:, qt, :], ALU.is_ge, ALU.mult,
                           reads=[B_sel, B_c], writes=[B_sel])
                    if sel_dbg is not None:
                        kb.cp(dve, sel_dbg[:, qt, g, :], sel[:], reads=[B_sel], writes=[B_sel])
                    kb.ts(dve, nsel[:], sel[:], 1.0, -NEG, ALU.subtract, ALU.mult, reads=[B_sel], writes=[B_sel])
                    ni = (2 * qt + g) % 2
                    kb.tr(psT[0][0:32, 0:128], nsel[:], ident_b[:], reads=[B_sel, B_const], writes=[B_psT[0]])
                    kb.cp(dve, nselT[ni][0:32, :], psT[0][0:32, 0:128], reads=[B_psT[0]], writes=[B_nselT[ni]])
                    kts = [(kt, trineg if kt == qt else None) for kt in range(qt + 1)]
                    attn_branch(qt, g, kts, ksT, vaug_s, 1, ac, Bac, nselT[ni], B_nselT[ni], psA[4], B_psA[4])
                    kts = []
                    for kt in (qt - 2, qt - 1, qt):
                        if kt < 0:
                            continue
                        kts.append((kt, trineg if kt == qt else (trineg2 if kt == qt - 2 else None)))
                    attn_branch(qt, g, kts, kwT, vaug_w, 2, ac, Bac, None, None, psA[5], B_psA[5])
                kb.cp(pool, onb[:], ac[:], reads=[Bac], writes=[B_onb])
                for c in range(4):
                    kb.tr(psT[1][:, c * 128:(c + 1) * 128], onb[:, c * 128:(c + 1) * 128], ident_b[:],
                          reads=[B_onb, B_const], writes=[B_psT[1]])
                kb.cp(dve, onsaT[:, :, qs_], psT[1][:, 0:512].rearrange("p (c q) -> p c q", c=4),
                      reads=[B_psT[1]], writes=[B_onsaT])
            if sel_dbg is not None:
                tap("sel", sel_dbg[:], [128, NT, 2, 32], F32, reads=[B_sel])
            if score_dbg is not None:
                tap("score", score_dbg[:], [128, NT, 2, 32], F32, reads=[B_sel])
            tap("onsaT", onsaT[:], [128, 4, S], BF16, reads=[B_onsaT])
            kb.barrier()

    convw_d = kb.dram_in("convwT", [128, 4, 4])
    convb_d = kb.dram_in("convbT", [128, 4])
    wq_d = kb.dram_in("ml_wq", [4, 128, 128])
    wk_d = kb.dram_in("ml_wk", [4, 128, 128])
    wv_d = kb.dram_in("ml_wv", [4, 128, 128])
    ngbc_d = kb.dram_in("ng_bc", [128, 512])
    LN_EPS = 1e-5

    def mlstm_phase(b, hT, bfmT, btm, fb_bc, ymlT, B_ymlT, B_b):
        with contextlib.ExitStack() as st:
            xmT = kb.sb(st, "xmT", [128, 4, S + 4], BF16)
            sig_o = kb.sb(st, "sig_o", [128, NT, 512], BF16)
            ifp = kb.sb(st, "ifp", [128, NT, 8], F32)
            B_xm, B_so, B_if = Buf("xm"), Buf("so"), Buf("if")
            kb.memset(pool, xmT[:, :, 0:3], 0.0, writes=[B_xm])
            with contextlib.ExitStack() as st2:
                wfm = kb.sb(st2, "wfmB", [128, KC, 4 * 128], BF16)
                wtm = kb.sb(st2, "wtmB", [128, KC, 520], BF16)
                B_w = Buf("wB")
                wfm_v = wfm_d.rearrange("(k p) n -> p k n", p=128)
                wtm_v = wtm_d.rearrange("(k p) n -> p k n", p=128)
                kb.dma(qg, wfm[:], wfm_v[:, :, 10 * 128:14 * 128], writes=[B_w])
                kb.dma(qg, wtm[:], wtm_v[:, :, 280:800], writes=[B_w])
                n = 0
                for j in range(4):
                    for tg in range(4):
                        pi = n % 4
                        n += 1
                        pb = psA[pi]
                        cs = slice(tg * 512, (tg + 1) * 512)
                        for k in range(KC):
                            kb.mm(pb[:], wfm[:, k, j * 128:(j + 1) * 128], hT[:, k, cs], start=(k == 0), stop=(k == KC - 1),
                                  reads=[B_w, B_hT], writes=[B_psA[pi]])
                        evac(xmT[:, j, 3 + tg * 512: 3 + (tg + 1) * 512], pb[:], bfmT[:, 10 + j:11 + j],
                             reads=[B_psA[pi], B_b], writes=[B_xm])
                for i in range(NT):
                    ts_ = slice(i * 128, (i + 1) * 128)
                    pi = 4 + i % 2
                    pa = psA[pi]
                    for k in range(KC):
                        kb.mm(pa[:], hT[:, k, ts_], wtm[:, k, 8:520], start=(k == 0), stop=False,
                              reads=[B_w, B_hT], writes=[B_psA[pi]])
                    kb.mm(pa[:], ones_r[:], btm[:, 288:800], start=False, stop=True, reads=[B_b, B_const], writes=[B_psA[pi]])
                    kb.actf(sig_o[:, i, :], pa[:], AF.Sigmoid, reads=[B_psA[pi]], writes=[B_so])
                    pc = psA[0 + i % 2]
                    for k in range(KC):
                        kb.mm(pc[:, 0:8], hT[:, k, ts_], wtm[:, k, 0:8], start=(k == 0), stop=False,
                              reads=[B_w, B_hT], writes=[B_psA[i % 2]])
                    kb.mm(pc[:, 0:8], ones_r[:], btm[:, 280:288], start=False, stop=True, reads=[B_b, B_const], writes=[B_psA[i % 2]])
                    kb.cp(dve, ifp[:, i, :], pc[:, 0:8], reads=[B_psA[i % 2]], writes=[B_if])
                kb.tt(dve, ifp[:, :, 4:8], ifp[:, :, 4:8], fb_bc[:, :].unsqueeze(1).to_broadcast([128, NT, 4]), ALU.add,
                      reads=[B_if, B_b], writes=[B_if])
                tap("xmT", xmT[:], [128, 4, S + 4], BF16, reads=[B_xm])
                tap("ifp", ifp[:], [128, NT, 8], F32, reads=[B_if])
                kb.barrier()
            B_c = Buf("mlconst")
            convw = kb.sb(st, "convw", [128, 4, 4], F32)
            convb = kb.sb(st, "convb", [128, 4], F32)
            wq = kb.sb(st, "wq", [128, 4, 128], BF16)
            wk = kb.sb(st, "wk", [128, 4, 128], BF16)
            wv = kb.sb(st, "wv", [128, 4, 128], BF16)
            ng_bc = kb.sb(st, "ng_bc", [128, 512], F32)
            tri_u = kb.sb(st, "tri_u", [128, 128], F32)
            trineg = kb.sb(st, "trinegm", [128, 128], BF16)
            ones_f = kb.sb(st, "ones_fm", [128, 128], F32)
            kb.dma(qs, convw[:], convw_d, writes=[B_c])
            kb.dma(qs, convb[:], convb_d, writes=[B_c])
            kb.dma(qs, ng_bc[:], ngbc_d, writes=[B_c])
            kb.dma(qs, tri_u[:], cdram["tri_u"], writes=[B_c])
            kb.dma(qg, trineg[:], cdram["trineg"], writes=[B_c])
            for t_, d_ in ((wq, wq_d), (wk, wk_d), (wv, wv_d)):
                kb.dma(qg, t_[:], d_.rearrange("h d e -> d h e"), writes=[B_c])
            kb.memset(dve, ones_f[:], 1.0, writes=[B_c])
            lf = kb.sb(st, "lf", [128, NT, 4], F32)
            F_tm = kb.sb(st, "F_tm", [128, NT, 4], F32)
            FL_b = kb.sb(st, "FL_b", [128, NT, 4], F32)
            bcol = kb.sb(st, "bcol", [128, NT, 4], F32)
            wi = kb.sb(st, "wi", [128, NT, 4], F32)
            gst = kb.sb(st, "gst", [128, NT, 4], F32)
            dec = kb.sb(st, "dec", [128, NT, 4], F32)
            B_g = Buf("gates")
            kb.actf(lf[:], ifp[:, :, 4:8], AF.Exp, scale=-1.0, reads=[B_if], writes=[B_g])
            kb.actf(lf[:], lf[:], AF.Ln, bias=1.0, reads=[B_g], writes=[B_g])
            kb.ts(dve, lf[:], lf[:], -1.0, None, ALU.mult, reads=[B_g], writes=[B_g])
            pF = psA[0]
            pL = psA[1]
            for i in range(NT):
                kb.mm(pF[:, i * 4:(i + 1) * 4], tri_u[:], lf[:, i, :], start=True, stop=True, reads=[B_c, B_g], writes=[B_psA[0]])
            for i in range(NT):
                kb.mm(pL[:, i * 4:(i + 1) * 4], ones_f[:], lf[:, i, :], start=True, stop=True, reads=[B_c, B_g], writes=[B_psA[1]])
            kb.cp(dve, F_tm[:], pF[:, 0:64].rearrange("p (i h) -> p i h", h=4), reads=[B_psA[0]], writes=[B_g])
            kb.cp(dve, FL_b[:], pL[:, 0:64].rearrange("p (i h) -> p i h", h=4), reads=[B_psA[1]], writes=[B_g])
            kb.tt(dve, bcol[:], ifp[:, :, 0:4], F_tm[:], ALU.subtract, reads=[B_if, B_g], writes=[B_g])
            kb.actf(wi[:], F_tm[:], AF.Exp, reads=[B_g], writes=[B_g])
            kb.tt(dve, gst[:], FL_b[:], bcol[:], ALU.add, reads=[B_g], writes=[B_g])
            kb.actf(gst[:], gst[:], AF.Exp, reads=[B_g], writes=[B_g])
            kb.actf(dec[:], FL_b[:], AF.Exp, reads=[B_g], writes=[B_g])
            tmpc = kb.sb(st, "tmpc", [128, S], F32)
            xcT = kb.sb(st, "xcT", [128, S], BF16)
            qmT = kb.sb(st, "qmT", [128, S], BF16)
            kmT = kb.sb(st, "kmT", [128, S], BF16)
            k_tm = kb.sb(st, "k_tm", [128, NT, 128], BF16)
            vaug = kb.sb(st, "vaug_m", [128, NT, 129], BF16)
            yml = kb.sb(st, "yml", [128, NT, 512], BF16)
            lfbc = [kb.sb(st, "lfbc%d" % i, [128, 128], F32) for i in range(2)]
            DT = [kb.sb(st, "DT%d" % i, [128, 128], F32) for i in range(2)]
            AT = [kb.sb(st, "AT%d" % i, [128, 128], BF16) for i in range(2)]
            p2s = [kb.sb(st, "p2s%d" % i, [128, 129], F32) for i in range(2)]
            nd = [kb.sb(st, "nd%d" % i, [128, 129], F32) for i in range(2)]
            hout = [kb.sb(st, "hout%d" % i, [128, 128], F32) for i in range(2)]
            kg = [kb.sb(st, "kg%d" % i, [128, 128], BF16) for i in range(2)]
            C_f = kb.sb(st, "C_f", [128, 129], F32)
            C_b = [kb.sb(st, "C_b%d" % i, [128, 129], BF16) for i in range(2)]
            sm = [kb.sb(st, "sm%d" % i, [128, 16], F32) for i in range(2)]
            B_tmpc, B_xc, B_qk, B_ktm, B_va, B_yml, B_Cf = Buf("tmpc"), Buf("xc"), Buf("qk"), Buf("ktm"), Buf("va"), Buf("yml"), Buf("Cf")
            B_w2 = [{nm: Buf(nm + str(i)) for nm in ("lfbc", "DT", "AT", "p2s", "nd", "hout", "kg", "Cb", "sm")} for i in range(2)]
            kb.memset(pool, vaug[:, :, 128:129], 1.0, writes=[B_va])
            SC = float(128 ** -0.5)
            for h in range(4):
                kb.ts(dve, tmpc[:], xmT[:, h, 0:S], convw[:, h, 0:1], None, ALU.mult, reads=[B_xm, B_c], writes=[B_tmpc])
                for k in range(1, 4):
                    kb.stt(dve, tmpc[:], xmT[:, h, k:k + S], convw[:, h, k:k + 1], tmpc[:], ALU.mult, ALU.add,
                           reads=[B_xm, B_c, B_tmpc], writes=[B_tmpc])
                kb.actf(xcT[:], tmpc[:], AF.Silu, bias=convb[:, h:h + 1], reads=[B_tmpc, B_c], writes=[B_xc])
                for tg in range(4):
                    cs = slice(tg * 512, (tg + 1) * 512)
                    kb.mm(psA[2][:], wq[:, h, :], xcT[:, cs], start=True, stop=True, reads=[B_c, B_xc], writes=[B_psA[2]])
                    kb.cp(dve, qmT[:, cs], psA[2][:], reads=[B_psA[2]], writes=[B_qk])
                    kb.mm(psA[3][:], wk[:, h, :], xcT[:, cs], start=True, stop=True, reads=[B_c, B_xc], writes=[B_psA[3]])
                    kb.actf(kmT[:, cs], psA[3][:], AF.Copy, scale=SC, reads=[B_psA[3]], writes=[B_qk])
                    for t4 in range(4):
                        i = tg * 4 + t4
                        kb.mm(psA[4][:, t4 * 128:(t4 + 1) * 128], xcT[:, i * 128:(i + 1) * 128], wk[:, h, :],
                              start=(t4 == 0), stop=(t4 == 3), reads=[B_c, B_xc], writes=[B_psA[4]])
                        kb.mm(psA[5][:, t4 * 128:(t4 + 1) * 128], xmT[:, h, 3 + i * 128: 3 + (i + 1) * 128], wv[:, h, :],
                              start=(t4 == 0), stop=(t4 == 3), reads=[B_c, B_xm], writes=[B_psA[5]])
                    kb.actf(k_tm[:, tg * 4:(tg + 1) * 4, :], psA[4][:].rearrange("p (t d) -> p t d", t=4), AF.Copy, scale=SC,
                            reads=[B_psA[4]], writes=[B_ktm])
                    kb.cp(dve, vaug[:, tg * 4:(tg + 1) * 4, 0:128], psA[5][:].rearrange("p (t d) -> p t d", t=4),
                          reads=[B_psA[5]], writes=[B_va])
                for i in range(NT):
                    w = i % 2
                    Bw = B_w2[w]
                    ts_ = slice(i * 128, (i + 1) * 128)
                    kb.cp(pool, lfbc[w][:], lf[:, i, h:h + 1].to_broadcast([128, 128]), reads=[B_g], writes=[Bw["lfbc"]])
                    pd = psA[0 + w]
                    kb.mm(pd[:, 0:128], ident_b[:], trineg[:], start=True, stop=False, reads=[B_c, B_const], writes=[B_psA[w]])
                    kb.mm(pd[:, 0:128], lfbc[w][:], tri_u[:], start=False, stop=True, reads=[Bw["lfbc"], B_c], writes=[B_psA[w]])
                    kb.actf(DT[w][:], pd[:, 0:128], AF.Exp, bias=bcol[:, i, h:h + 1], reads=[B_psA[w], B_g], writes=[Bw["DT"]])
                    pq = psA[2 + w]
                    kb.mm(pq[:, 0:128], kmT[:, ts_], qmT[:, ts_], start=True, stop=True, reads=[B_qk], writes=[B_psA[2 + w]])
                    kb.tt(dve, AT[w][:], pq[:, 0:128], DT[w][:], ALU.mult, reads=[B_psA[2 + w], Bw["DT"]], writes=[Bw["AT"]])
                    p2 = psA[4]
                    kb.mm(p2[:, 0:129], AT[w][:], vaug[:, i, :], start=True, stop=True, reads=[Bw["AT"], B_va], writes=[B_psA[4]])
                    if i == 0:
                        kb.cp(dve, nd[w][:], p2[:, 0:129], reads=[B_psA[4]], writes=[Bw["nd"]])
                    else:
                        kb.actf(p2s[w][:], p2[:, 0:129], AF.Copy, reads=[B_psA[4]], writes=[Bw["p2s"]])
                        p1 = psA[5]
                        cb_ = C_b[(i - 1) % 2]
                        kb.mm(p1[:, 0:129], qmT[:, ts_], cb_[:], start=True, stop=True,
                              reads=[B_qk, B_w2[(i - 1) % 2]["Cb"]], writes=[B_psA[5]])
                        kb.stt(dve, nd[w][:], p1[:, 0:129], wi[:, i, h:h + 1], p2s[w][:], ALU.mult, ALU.add,
                               reads=[B_psA[5], B_g, Bw["p2s"]], writes=[Bw["nd"]])
                    kb.ts(dve, sm[w][:, 0:1], nd[w][:, 128:129], -1.0, 1.0, ALU.mult, ALU.max, reads=[Bw["nd"]], writes=[Bw["sm"]])
                    kb.ts(dve, sm[w][:, 1:2], nd[w][:, 128:129], 1.0, None, ALU.max, reads=[Bw["nd"]], writes=[Bw["sm"]])
                    kb.tt(dve, sm[w][:, 0:1], sm[w][:, 0:1], sm[w][:, 1:2], ALU.max, reads=[Bw["sm"]], writes=[Bw["sm"]])
                    kb.op(dve, lambda: nc.vector.reciprocal(sm[w][:, 0:1], sm[w][:, 0:1]), reads=[Bw["sm"]], writes=[Bw["sm"]])
                    kb.ts(dve, hout[w][:], nd[w][:, 0:128], sm[w][:, 0:1], None, ALU.mult, reads=[Bw["nd"], Bw["sm"]], writes=[Bw["hout"]])
                    kb.op(dve, lambda: nc.vector.bn_stats(sm[w][:, 2:8], hout[w][:]), reads=[Bw["hout"]], writes=[Bw["sm"]])
                    kb.op(dve, lambda: nc.vector.bn_aggr(sm[w][:, 8:10], sm[w][:, 2:8]), reads=[Bw["sm"]], writes=[Bw["sm"]])
                    kb.ts(dve, sm[w][:, 10:11], sm[w][:, 9:10], LN_EPS, None, ALU.add, reads=[Bw["sm"]], writes=[Bw["sm"]])
                    kb.actf(sm[w][:, 10:11], sm[w][:, 10:11], AF.Sqrt, reads=[Bw["sm"]], writes=[Bw["sm"]])
                    kb.op(dve, lambda: nc.vector.reciprocal(sm[w][:, 10:11], sm[w][:, 10:11]), reads=[Bw["sm"]], writes=[Bw["sm"]])
                    kb.ts(dve, hout[w][:], hout[w][:], sm[w][:, 8:9], sm[w][:, 10:11], ALU.subtract, ALU.mult,
                          reads=[Bw["hout"], Bw["sm"]], writes=[Bw["hout"]])
                    kb.tt(dve, hout[w][:], hout[w][:], ng_bc[:, h * 128:(h + 1) * 128], ALU.mult, reads=[Bw["hout"], B_c], writes=[Bw["hout"]])
                    kb.tt(dve, yml[:, i, h * 128:(h + 1) * 128], hout[w][:], sig_o[:, i, h * 128:(h + 1) * 128], ALU.mult,
                          reads=[Bw["hout"], B_so], writes=[B_yml])
                    if i < NT - 1:
                        kb.ts(pool, kg[w][:], k_tm[:, i, :], gst[:, i, h:h + 1], None, ALU.mult, reads=[B_ktm, B_g], writes=[Bw["kg"]])
                        pcx = psA[5] if i == 0 else psA[4]
                        Bpc = B_psA[5] if i == 0 else B_psA[4]
                        kb.mm(pcx[:, 256:385], kg[w][:], vaug[:, i, :], start=True, stop=True, reads=[Bw["kg"], B_va], writes=[Bpc])
                        if i == 0:
                            kb.cp(dve, C_f[:], pcx[:, 256:385], reads=[Bpc], writes=[B_Cf])
                        else:
                            kb.stt(dve, C_f[:], C_f[:], dec[:, i, h:h + 1], pcx[:, 256:385], ALU.mult, ALU.add,
                                   reads=[B_Cf, B_g, Bpc], writes=[B_Cf])
                        kb.actf(C_b[w][:], C_f[:], AF.Copy, reads=[B_Cf], writes=[Bw["Cb"]])
            tap("yml", yml[:], [128, NT, 512], BF16, reads=[B_yml])
            for i in range(NT):
                p = i % 2
                for c in range(4):
                    kb.tr(psT[p][:, c * 128:(c + 1) * 128], yml[:, i, c * 128:(c + 1) * 128], ident_b[:],
                          reads=[B_yml, B_const], writes=[B_psT[p]])
                kb.cp(dve, ymlT[:, :, i * 128:(i + 1) * 128], psT[p][:, 0:512].rearrange("p (c q) -> p c q", c=4),
                      reads=[B_psT[p]], writes=[B_ymlT])
            kb.barrier()

    pa_d = kb.dram_in("proj_a", [512, D])
    pb_d = kb.dram_in("proj_b", [512, D])
    wo_d = kb.dram_in("w_out", [D, D])
    if stage >= 6:
        rw_d = kb.dram_in("router_w", [D, 32])
        rb_d = kb.dram_in("router_brow", [128, 32])
        wup_d = kb.dram_in("exp_w_up", [32, D, 2 * D])
        wdn_d = kb.dram_in("exp_w_down", [32, D, D])
        bupT_d = kb.dram_in("b_upT", [128, 32, 16])
        bdn_d = kb.dram_in("exp_b_down", [32, D])
        fg_d = kb.dram_in("fg_bc", [128, D])

    def merge_phase(b, hT, onsaT, ymlT, bfmT, B_b, B_onsaT, B_ymlT):
        with contextlib.ExitStack() as st:
            wmg = kb.sb(st, "wmg", [128, KC, 16 * 128], BF16)
            pa = kb.sb(st, "pa", [128, 4, D], BF16)
            pb_ = kb.sb(st, "pb", [128, 4, D], BF16)
            wo = kb.sb(st, "wo", [128, KC, D], BF16)
            g1 = kb.sb(st, "g1bc", [128, D], F32)
            B_w = Buf("wmerge")
            kb.dma(qg, wmg[:], wfm_d.rearrange("(k p) n -> p k n", p=128)[:, :, 14 * 128:30 * 128], writes=[B_w])
            kb.dma(qg, pa[:], pa_d.rearrange("(k p) n -> p k n", p=128), writes=[B_w])
            kb.dma(qg, pb_[:], pb_d.rearrange("(k p) n -> p k n", p=128), writes=[B_w])
            kb.dma(qg, wo[:], wo_d.rearrange("(k p) n -> p k n", p=128), writes=[B_w])
            kb.dma(qs, g1[:], gate_d[b, 0], reads=[B_gd], writes=[B_w])
            preT = kb.sb(st, "preT", [128, KC, 512], BF16)
            gsb = [kb.sb(st, "gsb%d" % i, [128, 512], BF16) for i in range(2)]
            ta = kb.sb(st, "ta", [128, 512], F32)
            tb = kb.sb(st, "tb", [128, 512], F32)
            xb = [kb.sb(st, "xb4_%d" % i, [128, D], F32) for i in range(2)]
            x1 = [kb.sb(st, "x1_%d" % i, [128, D], F32) for i in range(2)]
            xn = [kb.sb(st, "xn4_%d" % i, [128, D], BF16) for i in range(2)]
            h2s = [kb.sb(st, "h2s%d" % i, [128, KC, 128], BF16) for i in range(2)]
            junk = kb.sb(st, "junk4", [128, D], BF16)
            ss = kb.sb(st, "ss4", [128, NT], F32)
            B_pre, B_ta, B_tb, B_junk, B_ss = Buf("pre"), Buf("ta"), Buf("tb"), Buf("junk4"), Buf("ss4")
            B_gsb = [Buf("gsb0"), Buf("gsb1")]
            B_xb = [Buf("xb0"), Buf("xb1")]
            B_x1 = [Buf("x1_0"), Buf("x1_1")]
            B_xn = [Buf("xn0"), Buf("xn1")]
            B_h2s = [Buf("h2s0"), Buf("h2s1")]
            for tg in range(4):
                cs = slice(tg * 512, (tg + 1) * 512)
                for fc in range(8):
                    for br in range(2):
                        src, Bsrc, pw = (onsaT, B_onsaT, pa) if br == 0 else (ymlT, B_ymlT, pb_)
                        pv = psA[0 + br]
                        for k in range(4):
                            kb.mm(pv[:], pw[:, k, fc * 128:(fc + 1) * 128], src[:, k, cs], start=(k == 0), stop=(k == 3),
                                  reads=[B_w, Bsrc], writes=[B_psA[br]])
                        pg = psA[2 + br]
                        gcol = br * 8 + fc
                        for k in range(KC):
                            kb.mm(pg[:], wmg[:, k, gcol * 128:(gcol + 1) * 128], hT[:, k, cs], start=(k == 0), stop=(k == KC - 1),
                                  reads=[B_w, B_hT], writes=[B_psA[2 + br]])
                        kb.actf(gsb[br][:], pg[:], AF.Sigmoid, bias=bfmT[:, 14 + gcol:15 + gcol],
                                reads=[B_psA[2 + br], B_b], writes=[B_gsb[br]])
                        if br == 0:
                            kb.tt(dve, ta[:], pv[:], gsb[0][:], ALU.mult, reads=[B_psA[0], B_gsb[0]], writes=[B_ta])
                        else:
                            kb.tt(dve, tb[:], pv[:], gsb[1][:], ALU.mult, reads=[B_psA[1], B_gsb[1]], writes=[B_tb])
                    kb.tt(pool, preT[:, fc, :], ta[:], tb[:], ALU.add, reads=[B_ta, B_tb], writes=[B_pre])
                for t4 in range(4):
                    i = tg * 4 + t4
                    p = i % 2
                    kb.dma(qs, xb[p][:], x_d[b, i * 128:(i + 1) * 128, :], writes=[B_xb[p]])
                    for hf in range(2):
                        pm = psA[4 + hf]
                        for k in range(KC):
                            kb.mm(pm[:], preT[:, k, t4 * 128:(t4 + 1) * 128], wo[:, k, hf * 512:(hf + 1) * 512],
                                  start=(k == 0), stop=(k == KC - 1), reads=[B_pre, B_w], writes=[B_psA[4 + hf]])
                        hs = slice(hf * 512, (hf + 1) * 512)
                        kb.tt(dve, x1[p][:, hs], pm[:], g1[:, hs], ALU.mult, reads=[B_psA[4 + hf], B_w], writes=[B_x1[p]])
                        kb.tt(dve, x1[p][:, hs], x1[p][:, hs], xb[p][:, hs], ALU.add, reads=[B_x1[p], B_xb[p]], writes=[B_x1[p]])
                    kb.dma(qs, x1_d[b, i * 128:(i + 1) * 128, :], x1[p][:], reads=[B_x1[p]], writes=[B_x1d])
                    kb.actf(junk[:], x1[p][:], AF.Square, accum_out=ss[:, i:i + 1], reads=[B_x1[p]], writes=[B_junk, B_ss])
                    kb.ts(dve, ss[:, i:i + 1], ss[:, i:i + 1], 1.0 / D, RMS_EPS, ALU.mult, ALU.add, reads=[B_ss], writes=[B_ss])
                    kb.actf(ss[:, i:i + 1], ss[:, i:i + 1], AF.Sqrt, reads=[B_ss], writes=[B_ss])
                    kb.op(dve, lambda: nc.vector.reciprocal(ss[:, i:i + 1], ss[:, i:i + 1]), reads=[B_ss], writes=[B_ss])
                    kb.ts(dve, xn[p][:], x1[p][:], ss[:, i:i + 1], None, ALU.mult, reads=[B_ss, B_x1[p]], writes=[B_xn[p]])
                    for c in range(KC):
                        kb.tr(psT[p][:, c * 128:(c + 1) * 128], xn[p][:, c * 128:(c + 1) * 128], ident_b[:],
                              reads=[B_xn[p], B_const], writes=[B_psT[p]])
                    for c in range(KC):
                        kb.actf(h2s[p][:, c, :], psT[p][:, c * 128:(c + 1) * 128], AF.Identity,
                                scale=s2T[:, c, b:b + 1], bias=modT[:, 24 + c, b:b + 1],
                                reads=[B_psT[p], B_mod], writes=[B_h2s[p]])
                    kb.dma(qs, h2T_d[b, :, :, i * 128:(i + 1) * 128], h2s[p][:], reads=[B_h2s[p]], writes=[B_h2d])
            kb.barrier()

    def moe_phase(b, nexp):
        with contextlib.ExitStack() as st:
            h2T = kb.sb(st, "h2T", [128, KC, S], BF16)
            acc = kb.sb(st, "acc", [128, NT, D], F32)
            gatew = kb.sb(st, "gatew", [128, NT, 32], F32)
            rw = kb.sb(st, "rw", [128, KC, 32], BF16)
            rb = kb.sb(st, "rb", [128, 32], BF16)
            bupT = kb.sb(st, "bupT", [128, 32, 16], F32)
            B_h2, B_acc, B_gw, B_c = Buf("h2T"), Buf("acc"), Buf("gatew"), Buf("moeconst")
            kb.dma(qs, h2T[:], h2T_d[b], reads=[B_h2d], writes=[B_h2])
            kb.dma(qg, rw[:], rw_d.rearrange("(k p) n -> p k n", p=128), writes=[B_c])
            kb.dma(qg, rb[:], rb_d, writes=[B_c])
            kb.dma(qs, bupT[:], bupT_d, writes=[B_c])
            lg = [kb.sb(st, "lg%d" % i, [128, 32], F32) for i in range(2)]
            ex = [kb.sb(st, "ex%d" % i, [128, 32], F32) for i in range(2)]
            t8 = [kb.sb(st, "t8%d" % i, [128, 12], F32) for i in range(2)]
            B_r = [Buf("r0"), Buf("r1")]
            gwT = kb.sb(st, "gwT", [128, S], BF16)
            bdall = kb.sb(st, "bdall", [128, D], BF16)
            B_gwT, B_bd = Buf("gwT"), Buf("bdall")
            kb.memset(pool, gwT[:], 0.0, writes=[B_gwT])
            kb.memset(pool, bdall[:], 0.0, writes=[B_bd])
            kb.dma(qg, bdall[0:32, :], bdn_d, writes=[B_bd])
            for i in range(NT):
                p = i % 2
                pr = psA[p]
                for k in range(KC):
                    kb.mm(pr[:, 0:32], h2T[:, k, i * 128:(i + 1) * 128], rw[:, k, :], start=(k == 0), stop=False,
                          reads=[B_h2, B_c], writes=[B_psA[p]])
                kb.mm(pr[:, 0:32], ones_r[:], rb[:], start=False, stop=True, reads=[B_c, B_const], writes=[B_psA[p]])
                kb.cp(dve, lg[p][:], pr[:, 0:32], reads=[B_psA[p]], writes=[B_r[p]])
                kb.op(dve, lambda: nc.vector.max(out=t8[p][:, 0:8], in_=lg[p][:]), reads=[B_r[p]], writes=[B_r[p]])
                kb.ts(dve, t8[p][:, 8:9], t8[p][:, 0:1], -1.0, None, ALU.mult, reads=[B_r[p]], writes=[B_r[p]])
                kb.actf(ex[p][:], lg[p][:], AF.Exp, bias=t8[p][:, 8:9], reads=[B_r[p]], writes=[B_r[p]])
                kb.stt(dve, ex[p][:], lg[p][:], t8[p][:, 3:4], ex[p][:], ALU.is_ge, ALU.mult, reads=[B_r[p]], writes=[B_r[p]])
                kb.op(dve, lambda: nc.vector.reduce_sum(t8[p][:, 9:10], ex[p][:], AX.X), reads=[B_r[p]], writes=[B_r[p]])
                kb.op(dve, lambda: nc.vector.reciprocal(t8[p][:, 9:10], t8[p][:, 9:10]), reads=[B_r[p]], writes=[B_r[p]])
                kb.ts(dve, gatew[:, i, :], ex[p][:], t8[p][:, 9:10], None, ALU.mult, reads=[B_r[p]], writes=[B_gw])
                kb.tr(psA[2 + p][0:32, 0:128], gatew[:, i, :], ident_f[:], reads=[B_gw, B_const], writes=[B_psA[2 + p]])
                kb.cp(dve, gwT[0:32, i * 128:(i + 1) * 128], psA[2 + p][0:32, 0:128], reads=[B_psA[2 + p]], writes=[B_gwT])
            tap("gatew", gatew[:], [128, NT, 32], F32, reads=[B_gw])
            aT = kb.sb(st, "aT", [128, KC, S], BF16)
            NRING = 5
            ring = [kb.sb(st, "ring%d" % i, [128, KC, 512], BF16) for i in range(NRING)]
            B_ring = [Buf("ring%d" % i) for i in range(NRING)]
            gc = [kb.sb(st, "gc%d" % i, [128, 512], F32) for i in range(2)]
            sg = [kb.sb(st, "sg%d" % i, [128, 512], F32) for i in range(2)]
            l1 = [kb.sb(st, "l1%d" % i, [128, 512], F32) for i in range(2)]
            B_wk = [{nm: Buf(nm + str(i)) for nm in ("gc", "sg", "l1")} for i in range(2)]
            B_aT = [Buf("aT%d" % i) for i in range(4)]
            pendC = []
            wup_v = wup_d.rearrange("e (k p) n -> e p k n", p=128)
            wdn_v = wdn_d.rearrange("e (k p) n -> e p k n", p=128)
            rn = [0]

            def load_piece(e, pc):
                r = rn[0] % NRING
                rn[0] += 1
                if pc < 4:
                    kb.dma(qg, ring[r][:, :, 0:256], wup_v[e, :, :, pc * 256:(pc + 1) * 256], writes=[B_ring[r]])
                    kb.dma(qg, ring[r][:, :, 256:512], wup_v[e, :, :, D + pc * 256: D + (pc + 1) * 256], writes=[B_ring[r]])
                else:
                    hf = pc - 4
                    kb.dma(qg, ring[r][:], wdn_v[e, :, :, hf * 512:(hf + 1) * 512], writes=[B_ring[r]])
                return r

            pending = []
            order = [(e, pc) for e in range(nexp) for pc in range(6)]
            nxt = [0]

            def prefetch(upto):
                while nxt[0] < len(order) and nxt[0] < upto:
                    e_, pc_ = order[nxt[0]]
                    pending.append(load_piece(e_, pc_))
                    nxt[0] += 1

            prefetch(3)
            na = 0
            for e in range(nexp):
                slots = []
                for qq in range(4):
                    prefetch(e * 6 + qq + 3)
                    r = pending.pop(0)
                    slots.append(r)
                    for tg in range(4):
                        cs = slice(tg * 512, (tg + 1) * 512)
                        for pr_ in range(2):
                            w = na % 2
                            na += 1
                            Bw = B_wk[w]
                            ch = 2 * qq + pr_
                            pg = psA[0 + w]
                            pl = psA[2 + w]
                            for k in range(KC):
                                kb.mm(pg[:], ring[r][:, k, pr_ * 128:(pr_ + 1) * 128], h2T[:, k, cs], start=(k == 0), stop=(k == KC - 1),
                                      reads=[B_ring[r], B_h2], writes=[B_psA[w]])
                            for k in range(KC):
                                kb.mm(pl[:], ring[r][:, k, 256 + pr_ * 128: 256 + (pr_ + 1) * 128], h2T[:, k, cs], start=(k == 0), stop=(k == KC - 1),
                                      reads=[B_ring[r], B_h2], writes=[B_psA[2 + w]])
                            kb.ts(dve, gc[w][:], pg[:], bupT[:, e, ch:ch + 1], 7.0, ALU.add, ALU.min,
                                  reads=[B_psA[w], B_c], writes=[Bw["gc"]])
                            kb.actf(sg[w][:], gc[w][:], AF.Sigmoid, scale=1.702, reads=[Bw["gc"]], writes=[Bw["sg"]])
                            kb.actf(l1[w][:], pl[:], AF.Identity, bias=bupT[:, e, 8 + ch:9 + ch], reads=[B_psA[2 + w], B_c], writes=[Bw["l1"]])
                            kb.ts(pool, l1[w][:], l1[w][:], 7.0, -7.0, ALU.min, ALU.max, reads=[Bw["l1"]], writes=[Bw["l1"]])
                            kb.tt(pool, sg[w][:], sg[w][:], gc[w][:], ALU.mult, reads=[Bw["sg"], Bw["gc"]], writes=[Bw["sg"]])
                            if pendC:
                                pendC.pop(0)()

                            def _fin(ch=ch, cs=cs, w=w, Bw=Bw, tg=tg):
                                kb.stt(dve, aT[:, ch, cs], l1[w][:], 1.0, sg[w][:], ALU.add, ALU.mult,
                                       reads=[Bw["l1"], Bw["sg"]], writes=[B_aT[tg]])
                            pendC.append(_fin)
                while pendC:
                    pendC.pop(0)()
                prefetch(e * 6 + 4 + 3)
                rd0 = pending.pop(0)
                prefetch(e * 6 + 5 + 3)
                rd1 = pending.pop(0)
                rds = [rd0, rd1]
                for i in range(NT):
                    for hf in range(2):
                        w = (i * 2 + hf) % 2
                        pd = psA[4 + w]
                        for k in range(KC):
                            kb.mm(pd[:], aT[:, k, i * 128:(i + 1) * 128], ring[rds[hf]][:, k, :], start=(k == 0), stop=(k == KC - 1),
                                  reads=[B_aT[i // 4], B_ring[rds[hf]]], writes=[B_psA[4 + w]])
                        av = acc[:, i, hf * 512:(hf + 1) * 512]
                        if e == 0:
                            kb.ts(dve, av, pd[:], gatew[:, i, e:e + 1], None, ALU.mult, reads=[B_psA[4 + w], B_gw], writes=[B_acc])
                        else:
                            kb.stt(dve, av, pd[:], gatew[:, i, e:e + 1], av, ALU.mult, ALU.add,
                                   reads=[B_psA[4 + w], B_gw, B_acc], writes=[B_acc])
            for i in range(NT):
                for hf in range(2):
                    w = (i * 2 + hf) % 2
                    pd = psA[4 + w]
                    kb.mm(pd[:], gwT[:, i * 128:(i + 1) * 128], bdall[:, hf * 512:(hf + 1) * 512], start=True, stop=True,
                          reads=[B_gwT, B_bd], writes=[B_psA[4 + w]])
                    av = acc[:, i, hf * 512:(hf + 1) * 512]
                    kb.tt(dve, av, av, pd[:], ALU.add, reads=[B_psA[4 + w], B_acc], writes=[B_acc])
            kb.barrier()
            g2 = ring[0][:].rearrange("p k n -> p (k n)")[:, 0:2 * D].bitcast(F32)
            fg = ring[1][:].rearrange("p k n -> p (k n)")[:, 0:2 * D].bitcast(F32)
            x1t = [ring[2][:].rearrange("p k n -> p (k n)")[:, 0:2 * D].bitcast(F32),
                   ring[3][:].rearrange("p k n -> p (k n)")[:, 0:2 * D].bitcast(F32)]
            ot = [aT[:, 0:2, :].rearrange("p k n -> p (k n)")[:, 0:2 * D].bitcast(F32),
                  aT[:, 2:4, :].rearrange("p k n -> p (k n)")[:, 0:2 * D].bitcast(F32)]
            junk = aT[:, 4, 0:D]
            B_f = Buf("fin")
            B_x1t = [Buf("x1t0"), Buf("x1t1")]
            B_ot = [Buf("ot0"), Buf("ot1")]
            ssf = kb.sb(st, "ssf", [128, NT], F32)
            kb.dma(qs, g2, gate_d[b, 1], reads=[B_gd], writes=[B_f])
            kb.dma(qs, fg, fg_d, writes=[B_f])
            for i in range(NT):
                p = i % 2
                kb.dma(qs, x1t[p], x1_d[b, i * 128:(i + 1) * 128, :], reads=[B_x1d], writes=[B_x1t[p]])
                kb.tt(dve, acc[:, i, :], acc[:, i, :], g2, ALU.mult, reads=[B_acc, B_f], writes=[B_acc])
                kb.tt(dve, x1t[p], x1t[p], acc[:, i, :], ALU.add, reads=[B_x1t[p], B_acc], writes=[B_x1t[p]])
                kb.actf(junk, x1t[p], AF.Square, accum_out=ssf[:, i:i + 1], reads=[B_x1t[p]], writes=[B_f])
                kb.ts(dve, ssf[:, i:i + 1], ssf[:, i:i + 1], 1.0 / D, RMS_EPS, ALU.mult, ALU.add, reads=[B_f], writes=[B_f])
                kb.actf(ssf[:, i:i + 1], ssf[:, i:i + 1], AF.Sqrt, reads=[B_f], writes=[B_f])
                kb.op(dve, lambda: nc.vector.reciprocal(ssf[:, i:i + 1], ssf[:, i:i + 1]), reads=[B_f], writes=[B_f])
                kb.stt(dve, ot[p], x1t[p], ssf[:, i:i + 1], fg, ALU.mult, ALU.mult, reads=[B_x1t[p], B_f], writes=[B_ot[p]])
                kb.dma(qs, out_d[b, i * 128:(i + 1) * 128, :], ot[p], reads=[B_ot[p]], writes=[B_out])
            kb.barrier()

    def mixer(b, sq):
        hT = kb.sb(sq, "hT%d" % b, [128, KC, S], BF16)
        bfmT = kb.sb(sq, "bfmT%d" % b, [128, N_FM], F32)
        btm = kb.sb(sq, "btm%d" % b, [128, TM_W], BF16)
        fb_bc = kb.sb(sq, "fb_bc%d" % b, [128, 4], F32)
        onsaT = kb.sb(sq, "onsaT%d" % b, [128, 4, S], BF16)
        B_onsaT = Buf("onsaT")
        B_ymlT = Buf("ymlT")
        B_b = Buf("bias")
        kb.dma(qs, bfmT[:], bfmT_d, writes=[B_b])
        kb.dma(qg, btm[:], btm_d, writes=[B_b])
        kb.dma(qs, fb_bc[:], fbias_d, writes=[B_b])
        with contextlib.ExitStack() as st:
            xb = [kb.sb(st, "xb%d" % i, [128, D], F32) for i in range(2)]
            xn = [kb.sb(st, "xn%d" % i, [128, D], BF16) for i in range(2)]
            junk = kb.sb(st, "junk", [128, D], BF16)
            ss = kb.sb(st, "ss", [128, NT], F32)
            rstd = kb.sb(st, "rstd", [128, NT], F32)
            B_x = [Buf("x0"), Buf("x1")]
            B_xn = [Buf("xn0"), Buf("xn1")]
            B_st = Buf("stats")
            B_junk = Buf("junk")
            for i in range(NT):
                p = i % 2
                kb.dma(qs, xb[p][:], x_d[b, i * 128:(i + 1) * 128, :], writes=[B_x[p]])
                kb.actf(junk[:], xb[p][:], AF.Square, accum_out=ss[:, i:i + 1], reads=[B_x[p]], writes=[B_junk, B_st])
                kb.ts(dve, rstd[:, i:i + 1], ss[:, i:i + 1], 1.0 / D, RMS_EPS, ALU.mult, ALU.add, reads=[B_st], writes=[B_st])
                kb.actf(rstd[:, i:i + 1], rstd[:, i:i + 1], AF.Sqrt, reads=[B_st], writes=[B_st])
                kb.op(dve, lambda: nc.vector.reciprocal(rstd[:, i:i + 1], rstd[:, i:i + 1]), reads=[B_st], writes=[B_st])
                kb.ts(dve, xn[p][:], xb[p][:], rstd[:, i:i + 1], None, ALU.mult, reads=[B_st, B_x[p]], writes=[B_xn[p]])
                for c in range(KC):
                    kb.tr(psT[p][:, c * 128:(c + 1) * 128], xn[p][:, c * 128:(c + 1) * 128], ident_b[:],
                          reads=[B_xn[p], B_const], writes=[B_psT[p]])
                for c in range(KC):
                    kb.actf(hT[:, c, i * 128:(i + 1) * 128], psT[p][:, c * 128:(c + 1) * 128], AF.Identity,
                            scale=s1T[:, c, b:b + 1], bias=modT[:, c, b:b + 1],
                            reads=[B_psT[p], B_mod], writes=[B_hT])
            tap("hT", hT[:], [128, KC, S], BF16, reads=[B_hT])
            kb.barrier()

        nsa = contextlib.ExitStack()
        qT = kb.sb(nsa, "qT", [128, 4, S], BF16)
        kcT = [kb.sb(nsa, "kcT%d" % g, [128, S], BF16) for g in range(2)]
        vcT = [kb.sb(nsa, "vcT%d" % g, [128, S], BF16) for g in range(2)]
        ksT = [[kb.sb(nsa, "ksT%d%d" % (g, v), [128, S], BF16) for v in range(2)] for g in range(2)]
        kwT = [[kb.sb(nsa, "kwT%d%d" % (g, v), [128, S], BF16) for v in range(2)] for g in range(2)]
        vaug_s = kb.sb(nsa, "vaug_s", [128, NT, 2, 65], BF16)
        vaug_w = kb.sb(nsa, "vaug_w", [128, NT, 2, 65], BF16)
        sig_nsa = kb.sb(nsa, "sig_nsa", [128, NT, 24], F32)
        B_q, B_kc, B_ks, B_kw, B_v, B_sn = Buf("q"), Buf("kc"), Buf("ks"), Buf("kw"), Buf("v"), Buf("sn")
        for g in range(2):
            zr = slice(64, 128) if g == 0 else slice(0, 64)
            kb.memset(pool, kcT[g][zr, :], 0.0, writes=[B_kc])
            kb.memset(pool, vcT[g][zr, :], 0.0, writes=[B_kc])
            for t_, Bt in ((ksT, B_ks), (kwT, B_kw)):
                kb.memset(pool, t_[g][0][64:128, :], 0.0, writes=[Bt])
                kb.memset(pool, t_[g][1][0:64, :], 0.0, writes=[Bt])
        kb.memset(pool, vaug_s[:, :, :, 64:65], 1.0, writes=[B_v])
        kb.memset(pool, vaug_w[:, :, :, 64:65], 1.0, writes=[B_v])
        with contextlib.ExitStack() as st:
            B_pacc = Buf("pacc")
            wfm = kb.sb(st, "wfmA", [128, KC, 10 * 128], BF16)
            wtm = kb.sb(st, "wtmA", [128, KC, 280], BF16)
            B_w = Buf("wA")
            wfm_v = wfm_d.rearrange("(k p) n -> p k n", p=128)
            wtm_v = wtm_d.rearrange("(k p) n -> p k n", p=128)
            kb.dma(qg, wfm[:], wfm_v[:, :, 0:10 * 128], writes=[B_w])
            kb.dma(qg, wtm[:], wtm_v[:, :, 0:280], writes=[B_w])
            n = 0
            for j in range(10):
                for tg in range(4):
                    pi = n % 4
                    n += 1
                    pb = psA[pi]
                    cs = slice(tg * 512, (tg + 1) * 512)
                    for k in range(KC):
                        kb.mm(pb[:], wfm[:, k, j * 128:(j + 1) * 128], hT[:, k, cs], start=(k == 0), stop=(k == KC - 1),
                              reads=[B_w, B_hT], writes=[B_psA[pi]])
                    rd = [B_psA[pi], B_b]
                    if j < 4:
                        evac(qT[:, j, cs], pb[:], bfmT[:, j:j + 1], mul=0.125, reads=rd, writes=[B_q])
                    elif j in (4, 5):
                        tl = kcT if j == 4 else vcT
                        evac(tl[0][0:64, cs], pb[0:64, :], bfmT[0:64, j:j + 1], reads=rd, writes=[B_kc], force_dve=True)
                        evac(tl[1][64:128, cs], pb[64:128, :], bfmT[64:128, j:j + 1], reads=rd, writes=[B_kc], force_dve=True)
                    else:
                        tl, Bt = (ksT, B_ks) if j < 8 else (kwT, B_kw)
                        g = (j - 6) % 2
                        evac(tl[g][0][0:64, cs], pb[0:64, :], bfmT[0:64, j:j + 1], reads=rd, writes=[Bt], force_dve=True)
                        evac(tl[g][1][64:128, cs], pb[64:128, :], bfmT[64:128, j:j + 1], reads=rd, writes=[Bt], force_dve=True)
            for i in range(NT):
                pi = 4 + i % 2
                pa = psA[pi]
                ts_ = slice(i * 128, (i + 1) * 128)
                for k in range(KC):
                    kb.mm(pa[:, 0:280], hT[:, k, ts_], wtm[:, k, :], start=(k == 0), stop=False,
                          reads=[B_w, B_hT], writes=[B_psA[pi]])
                kb.mm(pa[:, 0:280], ones_r[:], btm[:, 0:280], start=False, stop=True, reads=[B_b, B_const], writes=[B_psA[pi]])
                kb.cp(dve, vaug_s[:, i, :, 0:64], pa[:, 0:128].rearrange("p (g d) -> p g d", g=2), reads=[B_psA[pi]], writes=[B_v, B_pacc])
                kb.cp(dve, vaug_w[:, i, :, 0:64], pa[:, 128:256].rearrange("p (g d) -> p g d", g=2), reads=[B_psA[pi]], writes=[B_v, B_pacc])
                kb.actf(sig_nsa[:, i, :], pa[:, 256:280], AF.Sigmoid, reads=[B_psA[pi]], writes=[B_sn, B_pacc])
            tap("qT", qT[:], [128, 4, S], BF16, reads=[B_q])
            tap("ksT00", ksT[0][0][:], [128, S], BF16, reads=[B_ks])
            tap("kwT11", kwT[1][1][:], [128, S], BF16, reads=[B_kw])
            tap("kcT1", kcT[1][:], [128, S], BF16, reads=[B_kc])
            tap("vaug_s", vaug_s[:], [128, NT, 2, 65], BF16, reads=[B_v])
            tap("sig_nsa", sig_nsa[:], [128, NT, 24], F32, reads=[B_sn])
            kb.barrier()
        if stage <= 2:
            nsa.close()
            return
        nsa_phase(b, qT, kcT, vcT, ksT, kwT, vaug_s, vaug_w, sig_nsa, onsaT, B_onsaT,
                  [B_q, B_kc, B_ks, B_kw, B_v, B_sn])
        nsa.close()
        if stage <= 3:
            return
        ymlT = kb.sb(sq, "ymlT%d" % b, [128, 4, S], BF16)
        mlstm_phase(b, hT, bfmT, btm, fb_bc, ymlT, B_ymlT, B_b)
        if stage <= 4:
            return
        merge_phase(b, hT, onsaT, ymlT, bfmT, B_b, B_onsaT, B_ymlT)

    B_out = Buf("out")
    for b in range(NB if stage > 7 else 1):
        with contextlib.ExitStack() as sq:
            mixer(b, sq)
            kb.barrier()
        if stage >= 6:
            moe_phase(b, 32 if stage >= 7 else 2)

    kb.barrier()
    return kb, tap_out


def prep_inputs(inputs):
    f = lambda a: np.ascontiguousarray(np.asarray(a, dtype=np.float32))
    x = f(inputs["x"])
    c = f(inputs["c"])
    w_in = f(inputs["w_in"][0])
    b_in = f(inputs["b_in"][0])
    fc = fm_cols()
    tc_ = tm_cols()
    shared = {
        "ada_w": f(inputs["ada_w"][0]),
        "ada_bT": colT(f(inputs["ada_b"][0]), 48),
        "n1gT": colT(f(inputs["norm1_g"][0]), KC),
        "n2gT": colT(f(inputs["norm2_g"][0]), KC),
        "w_fm": np.ascontiguousarray(w_in[:, fc.reshape(-1)]),
        "b_fmT": np.ascontiguousarray(b_in[fc].T),
        "w_tm": np.ascontiguousarray(w_in[:, tc_]),
    }
    brow = np.zeros((128, 2 * D), np.float32)
    ab = f(inputs["ada_b"][0])
    brow[0, :D] = ab[2 * D:3 * D]
    brow[0, D:] = ab[5 * D:6 * D]
    shared["ada_brow"] = brow
    btm = np.zeros((128, TM_W), np.float32)
    btm[0] = b_in[tc_]
    shared["b_tm"] = btm
    shared["fbias_bc"] = np.ascontiguousarray(np.broadcast_to(f(inputs["ml_f_bias"][0])[None, :], (128, 4)))
    for nm in ("cmp_w1_k", "cmp_w1_v", "cmp_w2_k", "cmp_w2_v"):
        shared[nm] = f(inputs[nm][0])
    for nm, src in (("peT_k", "cmp_pe_k"), ("peT_v", "cmp_pe_v")):
        pt = np.zeros((128, 32), np.float32)
        pt[0:64] = f(inputs[src][0]).T
        pt[64:128] = 0.0
        shared[nm] = pt
    cw = f(inputs["ml_conv_w"][0])
    shared["convwT"] = np.ascontiguousarray(cw.reshape(4, 4, 128).transpose(2, 1, 0))
    shared["convbT"] = colT(f(inputs["ml_conv_b"][0]), 4)
    for nm in ("ml_wq", "ml_wk", "ml_wv"):
        shared[nm] = f(inputs[nm][0])
    shared["ng_bc"] = np.ascontiguousarray(np.broadcast_to(f(inputs["ml_norm_g"][0])[None, :], (128, 512)))
    for nm in ("proj_a", "proj_b", "w_out", "router_w", "exp_w_up", "exp_w_down", "exp_b_down"):
        shared[nm] = f(inputs[nm][0])
    rbr = np.zeros((128, 32), np.float32)
    rbr[0] = f(inputs["router_b"][0])
    shared["router_brow"] = rbr
    bu = f(inputs["exp_b_up"][0])
    shared["b_upT"] = np.ascontiguousarray(bu.reshape(32, 16, 128).transpose(2, 0, 1))
    shared["fg_bc"] = np.ascontiguousarray(np.broadcast_to(f(inputs["final_g"])[None, :], (128, D)))
    shared.update(host_consts())
    maps = []
    for i in range(NCORES):
        m = dict(shared)
        m["x"] = np.ascontiguousarray(x[i * NB:(i + 1) * NB])
        cl = c[i * NB:(i + 1) * NB]
        m["cT"] = np.ascontiguousarray(cl.reshape(NB, KC, 128).transpose(2, 1, 0))
        maps.append(m)
    return maps


def kernel(**inputs):
    kb, _ = build(stage=99)
    maps = prep_inputs(inputs)
    res = run_bass_kernel_spmd(kb.nc, maps, core_ids=list(range(NCORES)))
    return np.concatenate([r["out"] for r in res.results], axis=0).astype(np.float32)
```

```python
import contextlib
import numpy as np
import ml_dtypes
import concourse.bass as bass
import concourse.mybir as mybir
from concourse.bass_utils import run_bass_kernel_spmd

F32 = mybir.dt.float32
BF16 = mybir.dt.bfloat16
AF = mybir.ActivationFunctionType
ALU = mybir.AluOpType
AX = mybir.AxisListType

NCORES = 8
D = 1024
S = 2048
NB = 2
NT = S // 128
KC = D // 128
NEG = -30000.0


class Buf:
    __slots__ = ("name", "last_w", "readers")

    def __init__(self, name):
        self.name = name
        self.last_w = None
        self.readers = {}


class Eng:
    def __init__(self, kb, name, h, is_pe=False):
        self.kb, self.name, self.h, self.is_pe = kb, name, h, is_pe
        self.sem = kb.new_sem("e_" + name)
        self.count = 0
        self.seen = {}

    def wait(self, tk):
        if tk is None:
            return
        sem, val = tk
        if sem is self.sem and self.is_pe:
            return
        if self.seen.get(sem, 0) >= val:
            return
        self.h.wait_ge(sem, val)
        self.seen[sem] = val


class DmaQ:
    def __init__(self, kb, name, eng, nsem=8):
        self.kb, self.eng = kb, eng
        self.sems = [kb.new_sem("d_%s%d" % (name, i)) for i in range(nsem)]
        self.vals = [0] * nsem
        self.n = 0


class KB:
    def __init__(self):
        self.nc = bass.Bass("TRN2", target_bir_lowering=False)
        self.root = contextlib.ExitStack()
        self._semn = 0
        self.in_names = []
        nc = self.nc
        self.pe = Eng(self, "pe", nc.tensor, is_pe=True)
        self.act = Eng(self, "act", nc.scalar)
        self.dve = Eng(self, "dve", nc.vector)
        self.pool = Eng(self, "pool", nc.gpsimd)
        self.sp = Eng(self, "sp", nc.sync)
        self.engs = [self.pe, self.act, self.dve, self.pool, self.sp]
        self.qs = DmaQ(self, "s", self.sp, 12)
        self.qg = DmaQ(self, "g", self.pool, 8)
        self.dqs = [self.qs, self.qg]

    def new_sem(self, name):
        self._semn += 1
        return self.root.enter_context(self.nc.semaphore("%s_%d" % (name, self._semn)))

    def dram_in(self, name, shape, dtype=F32):
        self.in_names.append(name)
        return self.nc.dram_tensor(name, list(shape), dtype, kind="ExternalInput").ap()

    def dram_out(self, name, shape, dtype=F32):
        return self.nc.dram_tensor(name, list(shape), dtype, kind="ExternalOutput").ap()

    def dram_tmp(self, name, shape, dtype=F32):
        return self.nc.dram_tensor(name, list(shape), dtype, kind="Internal").ap()

    def sb(self, stack, name, shape, dtype):
        self._semn += 1
        return stack.enter_context(self.nc.sbuf_tensor("sb%d_%s" % (self._semn, name), list(shape), dtype))

    def ps(self, stack, name, shape, dtype):
        self._semn += 1
        return stack.enter_context(self.nc.psum_tensor("pp%d_%s" % (self._semn, name), list(shape), dtype))

    def _deps(self, eng, reads, writes):
        for b in reads:
            eng.wait(b.last_w)
        for b in writes:
            eng.wait(b.last_w)
            for sem, val in b.readers.items():
                eng.wait((sem, val))

    def _commit(self, tk, reads, writes):
        for b in reads:
            sem, val = tk
            if b.readers.get(sem, 0) < val:
                b.readers[sem] = val
        for b in writes:
            b.last_w = tk
            b.readers = {}

    def op(self, eng, fn, reads=(), writes=()):
        self._deps(eng, reads, writes)
        ins = fn()
        eng.count += 1
        ins.then_inc(eng.sem, 1)
        tk = (eng.sem, eng.count)
        self._commit(tk, reads, writes)
        return tk

    def dma(self, q, out, in_, reads=(), writes=()):
        eng = q.eng
        self._deps(eng, reads, writes)
        slot = q.n % len(q.sems)
        q.n += 1
        sem = q.sems[slot]
        if q.vals[slot]:
            eng.wait((sem, q.vals[slot]))
        ins = eng.h.dma_start(out=out, in_=in_)
        ins.then_inc(sem, 16)
        q.vals[slot] += 16
        tk = (sem, q.vals[slot])
        self._commit(tk, reads, writes)
        return tk

    def barrier(self):
        tks = [(e.sem, e.count) for e in self.engs if e.count]
        for q in self.dqs:
            tks += [(s, v) for s, v in zip(q.sems, q.vals) if v]
        for e in self.engs:
            for tk in tks:
                if tk[0] is e.sem:
                    continue
                e.wait(tk)

    def mm(self, out, lhsT, rhs, start, stop, reads=(), writes=()):
        nc = self.nc
        return self.op(self.pe, lambda: nc.tensor.matmul(out, lhsT, rhs, start=start, stop=stop,
                                                         skip_group_check=True), reads, writes)

    def tr(self, out, in_, ident, reads=(), writes=()):
        nc = self.nc
        return self.op(self.pe, lambda: nc.tensor.transpose(out, in_, ident), reads, writes)

    def actf(self, out, in_, func, bias=None, scale=None, accum_out=None, reads=(), writes=()):
        nc = self.nc
        kw = {}
        if bias is not None:
            kw["bias"] = bias
        if scale is not None:
            kw["scale"] = scale
        if accum_out is not None:
            kw["accum_out"] = accum_out
        return self.op(self.act, lambda: nc.scalar.activation(out=out, in_=in_, func=func, **kw), reads, writes)

    def ts(self, eng, out, in0, s1, s2, op0, op1=None, reads=(), writes=()):
        kw = {}
        if op1 is not None:
            kw["op1"] = op1
        return self.op(eng, lambda: eng.h.tensor_scalar(out, in0, s1, s2, op0, **kw), reads, writes)

    def tt(self, eng, out, in0, in1, op, reads=(), writes=()):
        return self.op(eng, lambda: eng.h.tensor_tensor(out, in0, in1, op), reads, writes)

    def stt(self, eng, out, in0, scalar, in1, op0, op1, reads=(), writes=()):
        return self.op(eng, lambda: eng.h.scalar_tensor_tensor(out, in0, scalar, in1, op0, op1), reads, writes)

    def cp(self, eng, out, in_, reads=(), writes=()):
        return self.op(eng, lambda: eng.h.tensor_copy(out, in_), reads, writes)

    def memset(self, eng, ap, val, writes=()):
        return self.op(eng, lambda: eng.h.memset(ap, val), (), writes)


def host_consts():
    c = {}
    c["ident_f"] = np.eye(128, dtype=np.float32)
    slopes = (2.0 ** (-8.0 * np.arange(1, 9) / 8)).astype(np.float64)
    n = np.arange(128)[:, None, None]
    qt = np.arange(NT)[None, :, None]
    q = np.arange(128)[None, None, :]
    cm = ((16 * n + 31) <= (128 * qt + q)) & (n < 127)
    c["cmask"] = cm.astype(np.float32)
    nn = np.arange(128)[:, None, None]
    hh = np.arange(8)[None, :, None]
    qq = np.arange(NT)[None, None, :]
    cb = slopes[hh] * (16 * nn + 15.5 - 128 * qq)
    cb = np.where((nn <= 8 * qq + 6) & (nn < 127), cb, NEG)
    c["cbias"] = cb.astype(np.float32)
    cs = np.arange(127)[:, None] * 16
    ss = np.arange(32)[None, :] * 64
    ov = np.clip(np.minimum(cs + 32, ss + 64) - np.maximum(cs, ss), 0, None) / 32.0
    ovp = np.zeros((128, 32), np.float32)
    ovp[:127] = ov
    c["ov"] = ovp
    qv = np.arange(128)[:, None, None]
    qtv = np.arange(NT)[None, :, None]
    jv = np.arange(32)[None, None, :]
    cur = 2 * qtv + (qv >= 64)
    forced = (jv == cur) | (jv == 0)
    fut = jv > cur
    c["fadj"] = np.where(forced, 1e4, np.where(fut, -1e4, 0.0)).astype(np.float32)
    c["notfut"] = (~fut).astype(np.float32)
    ex = np.zeros((128, NT, 128), np.float32)
    for kt in range(NT):
        ex[2 * kt, kt, 0:64] = 1.0
        ex[2 * kt + 1, kt, 64:128] = 1.0
    c["expand"] = ex
    p = np.arange(128)[:, None]
    qc = np.arange(128)[None, :]
    c["trineg"] = np.where(p > qc, NEG, 0.0).astype(np.float32)
    c["trineg2"] = np.where(p <= qc, NEG, 0.0).astype(np.float32)
    pv = np.arange(128)[:, None, None]
    dl = np.arange(16)[None, None, :]
    c["alibi"] = (slopes[hh] * (pv - 128 * dl)).astype(np.float32)
    c["tri_u"] = (p <= qc).astype(np.float32)
    bf = ml_dtypes.bfloat16
    al = np.zeros((128, 2, 16, 128), np.float32)
    hind = np.zeros((128, 4, 128), np.float32)
    pp = np.arange(128)[None, :]
    dd = np.arange(16)[:, None]
    for g in range(2):
        for h4 in range(4):
            val = (slopes[4 * g + h4] * (pp - 128 * dd)).astype(np.float32)
            hi = val.astype(bf).astype(np.float32)
            lo = (val - hi).astype(bf).astype(np.float32)
            al[2 * h4, g] = hi
            al[2 * h4 + 1, g] = lo
    for h4 in range(4):
        hind[2 * h4:2 * h4 + 2, h4, :] = 1.0
    c["AL"] = al
    c["hind"] = hind
    return c


FM_Q, FM_KC, FM_VC, FM_KS0, FM_KS1, FM_KW0, FM_KW1, FM_XM, FM_MG = 0, 4, 5, 6, 7, 8, 9, 10, 14
N_FM = 30
N_FM1 = 14
TM_A = 288
TM_W = 800


def fm_cols():
    cols = []
    for c in range(4):
        cols.append(np.arange(c * 128, (c + 1) * 128))
    kv0 = 512
    cols.append(kv0 + np.arange(0, 128))
    cols.append(kv0 + np.arange(128, 256))
    for base in (256, 512):
        for g in range(2):
            a = kv0 + base + g * 64 + np.arange(64)
            cols.append(np.concatenate([a, a]))
    xm0 = 512 + 768 + 24
    for c in range(4):
        cols.append(xm0 + np.arange(c * 128, (c + 1) * 128))
    mg0 = xm0 + 512 + 512 + 8
    for c in range(16):
        cols.append(mg0 + np.arange(c * 128, (c + 1) * 128))
    return np.stack(cols)


def tm_cols():
    kv0 = 512
    xm0 = 512 + 768 + 24
    return np.concatenate([kv0 + 384 + np.arange(128), kv0 + 640 + np.arange(128),
                           512 + 768 + np.arange(24), xm0 + 1024 + np.arange(8),
                           xm0 + 512 + np.arange(512)])


def colT(v, n):
    return np.ascontiguousarray(v.reshape(n, 128).T)


def build(stage=99, taps=()):
    kb = KB()
    nc = kb.nc
    root = kb.root
    pe, act, dve, pool, sp = kb.pe, kb.act, kb.dve, kb.pool, kb.sp
    qs, qg = kb.qs, kb.qg
    taps = set(taps)
    tap_out = {}

    x_d = kb.dram_in("x", [NB, S, D])
    cT_d = kb.dram_in("cT", [128, KC, NB])
    adaw_d = kb.dram_in("ada_w", [D, 6 * D])
    adabT_d = kb.dram_in("ada_bT", [128, 48])
    adabrow_d = kb.dram_in("ada_brow", [128, 2 * D])
    n1gT_d = kb.dram_in("n1gT", [128, KC])
    n2gT_d = kb.dram_in("n2gT", [128, KC])
    wfm_d = kb.dram_in("w_fm", [D, N_FM * 128])
    bfmT_d = kb.dram_in("b_fmT", [128, N_FM])
    wtm_d = kb.dram_in("w_tm", [D, TM_W])
    btm_d = kb.dram_in("b_tm", [128, TM_W])
    identf_d = kb.dram_in("ident_f", [128, 128])
    out_d = kb.dram_out("out", [NB, S, D]) if stage >= 6 else None

    def tap(name, ap_sb, shape, dtype=F32, reads=()):
        if name not in taps:
            return
        d = kb.dram_out("tap_" + name, shape, dtype)
        tap_out[name] = d
        kb.dma(qs, d, ap_sb, reads=reads)

    ident_f = kb.sb(root, "ident_f", [128, 128], F32)
    ident_b = kb.sb(root, "ident_b", [128, 128], BF16)
    ones_r = kb.sb(root, "ones_r", [128, 128], BF16)
    ones_rf = kb.sb(root, "ones_rf", [128, 128], F32)
    modT = kb.sb(root, "modT", [128, 48, NB], F32)
    s1T = kb.sb(root, "s1T", [128, KC, NB], F32)
    s2T = kb.sb(root, "s2T", [128, KC, NB], F32)
    gate_d = kb.dram_tmp("gate_scr", [NB, 2, 128, D])
    x1_d = (kb.dram_out if "x1" in taps else kb.dram_tmp)("x1_scr", [NB, S, D])
    h2T_d = (kb.dram_out if "h2T" in taps else kb.dram_tmp)("h2T_scr", [NB, 128, KC, S], BF16)
    B_gd, B_x1d, B_h2d = Buf("gate_d"), Buf("x1_d"), Buf("h2T_d")
    B_const = Buf("const")
    B_mod = Buf("mod")

    psA = [kb.ps(root, "psA%d" % i, [128, 512], F32) for i in range(6)]
    psT = [kb.ps(root, "psT%d" % i, [128, 1024], BF16) for i in range(2)]
    B_psA = [Buf("psA%d" % i) for i in range(6)]
    B_psT = [Buf("psT%d" % i) for i in range(2)]

    kb.dma(qs, ident_f[:], identf_d, writes=[B_const])
    kb.cp(dve, ident_b[:], ident_f[:], reads=[B_const], writes=[B_const])
    kb.memset(dve, ones_r[:], 0.0, writes=[B_const])
    kb.memset(dve, ones_r[0:1, :], 1.0, writes=[B_const])
    kb.memset(dve, ones_rf[:], 0.0, writes=[B_const])
    kb.memset(dve, ones_rf[0:1, :], 1.0, writes=[B_const])

    with contextlib.ExitStack() as st:
        cT = kb.sb(st, "cT", [128, KC, NB], F32)
        scT = kb.sb(st, "scT", [128, KC, NB], F32)
        scbc = kb.sb(st, "scbc", [128, KC, NB, 128], F32)
        adabT = kb.sb(st, "adabT", [128, 48], F32)
        adabrow = kb.sb(st, "adabrow", [128, 2 * D], F32)
        n1gT = kb.sb(st, "n1gT", [128, KC], F32)
        n2gT = kb.sb(st, "n2gT", [128, KC], F32)
        awb = [kb.sb(st, "awb%d" % i, [128, KC, D], F32) for i in range(2)]
        gstage = [kb.sb(st, "gstage%d" % i, [128, 512], F32) for i in range(2)]
        B_gs = [Buf("gs0"), Buf("gs1")]
        B_aw = [Buf("aw0"), Buf("aw1")]
        B_c = Buf("c")
        kb.dma(qs, cT[:], cT_d, writes=[B_c])
        kb.dma(qs, adabT[:], adabT_d, writes=[B_c])
        kb.dma(qs, adabrow[:], adabrow_d, writes=[B_c])
        kb.dma(qs, n1gT[:], n1gT_d, writes=[B_c])
        kb.dma(qs, n2gT[:], n2gT_d, writes=[B_c])
        kb.actf(scT[:], cT[:], AF.Silu, reads=[B_c], writes=[B_c])
        for b in range(NB):
            for k in range(KC):
                kb.cp(dve, scbc[:, k, b, :], scT[:, k, b:b + 1].to_broadcast([128, 128]), reads=[B_c], writes=[B_c])
        adaw_v = adaw_d.rearrange("(k p) n -> p k n", p=128)
        pm = psA[0]
        first = True
        for grp in range(6):
            wb = awb[grp % 2]
            kb.dma(qs, wb[:], adaw_v[:, :, grp * D:(grp + 1) * D], writes=[B_aw[grp % 2]])
            for j in range(8):
                jj = grp * 8 + j
                for k in range(KC):
                    kb.mm(pm[:, jj * NB:(jj + 1) * NB], wb[:, k, j * 128:(j + 1) * 128], scT[:, k, :],
                          start=(k == 0), stop=(k == KC - 1),
                          reads=[B_aw[grp % 2], B_c], writes=[B_psA[0]])
            if grp in (2, 5):
                gi = 0 if grp == 2 else 1
                for b in range(NB):
                    for hf in range(2):
                        pb = psA[1 + hf]
                        for k in range(KC):
                            kb.mm(pb[:], scbc[:, k, b, :], wb[:, k, hf * 512:(hf + 1) * 512],
                                  start=(k == 0), stop=False, reads=[B_aw[grp % 2], B_c], writes=[B_psA[1 + hf]])
                        kb.mm(pb[:], ones_rf[:], adabrow[:, gi * D + hf * 512: gi * D + (hf + 1) * 512],
                              start=False, stop=True, reads=[B_c, B_const], writes=[B_psA[1 + hf]])
                        kb.cp(dve, gstage[hf][:], pb[:], reads=[B_psA[1 + hf]], writes=[B_gs[hf]])
                        kb.dma(qs, gate_d[b, gi, :, hf * 512:(hf + 1) * 512], gstage[hf][:], reads=[B_gs[hf]], writes=[B_gd])
        for b in range(NB):
            pv = pm[:, 0:48 * NB].rearrange("p (j b) -> p j b", b=NB)
            kb.tt(dve, modT[:, :, b], pv[:, :, b], adabT[:], ALU.add, reads=[B_psA[0], B_c], writes=[B_mod])
        for b in range(NB):
            kb.stt(dve, s1T[:, :, b], modT[:, 8:16, b], 1.0, n1gT[:], ALU.add, ALU.mult, reads=[B_mod, B_c], writes=[B_mod])
            kb.stt(dve, s2T[:, :, b], modT[:, 32:40, b], 1.0, n2gT[:], ALU.add, ALU.mult, reads=[B_mod, B_c], writes=[B_mod])
        tap("modT", modT[:], [128, 48, NB], reads=[B_mod])
        kb.barrier()

    fbias_d = kb.dram_in("fbias_bc", [128, 4])
    RMS_EPS = 1e-5
    B_hT = Buf("hT")

    evac_rr = [0]

    def evac(out, in_, bias, mul=None, reads=(), writes=(), force_dve=False):
        evac_rr[0] += 1
        if force_dve or evac_rr[0] % 2 == 0:
            if mul is None:
                kb.ts(dve, out, in_, bias, None, ALU.add, reads=reads, writes=writes)
            else:
                kb.ts(dve, out, in_, bias, mul, ALU.add, ALU.mult, reads=reads, writes=writes)
        else:
            if mul is None:
                kb.actf(out, in_, AF.Identity, bias=bias, reads=reads, writes=writes)
            else:
                kb.ts(dve, out, in_, bias, mul, ALU.add, ALU.mult, reads=reads, writes=writes)

    cdram = {}
    for nm, shp in (("cmask", [128, NT, 128]), ("cbias", [128, 8, NT]), ("ov", [128, 32]),
                    ("fadj", [128, NT, 32]), ("notfut", [128, NT, 32]), ("expand", [128, NT, 128]),
                    ("trineg", [128, 128]), ("trineg2", [128, 128]), ("alibi", [128, 8, 16]),
                    ("tri_u", [128, 128]), ("AL", [128, 2, 16, 128]), ("hind", [128, 4, 128])):
        cdram[nm] = kb.dram_in(nm, shp)
    w1k_d = kb.dram_in("cmp_w1_k", [32, 64, 128])
    w1v_d = kb.dram_in("cmp_w1_v", [32, 64, 128])
    w2k_d = kb.dram_in("cmp_w2_k", [128, 64])
    w2v_d = kb.dram_in("cmp_w2_v", [128, 64])
    pek_d = kb.dram_in("peT_k", [128, 32])
    pev_d = kb.dram_in("peT_v", [128, 32])

    def nsa_phase(b, qT, kcT, vcT, ksT, kwT, vaug_s, vaug_w, sig_nsa, onsaT, B_onsaT, B_in):
        with contextlib.ExitStack() as st:
            B_c = Buf("nsaconst")
            cmask = kb.sb(st, "cmask", [128, NT, 128], BF16)
            cbias = kb.sb(st, "cbias", [128, 8, NT], F32)
            ov = kb.sb(st, "ov", [128, 32], F32)
            fadj = kb.sb(st, "fadj", [128, NT, 32], F32)
            notfut = kb.sb(st, "notfut", [128, NT, 32], F32)
            expand = kb.sb(st, "expand", [128, NT, 128], BF16)
            trineg = kb.sb(st, "trineg", [128, 128], BF16)
            trineg2 = kb.sb(st, "trineg2", [128, 128], BF16)
            AL = kb.sb(st, "AL", [128, 2, 16, 128], BF16)
            hind = kb.sb(st, "hind", [128, 4, 128], BF16)
            ones_f = kb.sb(st, "ones_f", [128, 128], F32)
            kb.dma(qg, AL[:], cdram["AL"], writes=[B_c])
            kb.dma(qg, hind[:], cdram["hind"], writes=[B_c])
            for t_, nm in ((cbias, "cbias"), (ov, "ov"), (fadj, "fadj"), (notfut, "notfut")):
                kb.dma(qs, t_[:], cdram[nm], writes=[B_c])
            for t_, nm in ((cmask, "cmask"), (expand, "expand"), (trineg, "trineg"), (trineg2, "trineg2")):
                kb.dma(qg, t_[:], cdram[nm], writes=[B_c])
            kb.memset(dve, ones_f[:], 1.0, writes=[B_c])
            kcc = [[kb.sb(st, "kcc%d%d" % (g, v), [128, 128], BF16) for v in range(2)] for g in range(2)]
            vcc = [kb.sb(st, "vcc%d" % g, [128, 64], BF16) for g in range(2)]
            st_outer = st
            st = contextlib.ExitStack()
            w1 = [kb.sb(st, "w1_%d" % i, [128, 32, 128], BF16) for i in range(2)]
            w2kd = kb.sb(st, "w2kd", [128, 128], BF16)
            w2v = kb.sb(st, "w2v", [128, 64], BF16)
            peT = [kb.sb(st, "peT%d" % i, [128, 32], BF16) for i in range(2)]
            for i, wd in enumerate((w1k_d, w1v_d)):
                v_ = wd.rearrange("l d e -> d l e")
                kb.dma(qg, w1[i][0:64, :, :], v_, writes=[B_c])
                kb.dma(qg, w1[i][64:128, :, :], v_, writes=[B_c])
            kb.dma(qg, w2kd[:, 0:64], w2k_d, writes=[B_c])
            kb.dma(qg, w2kd[:, 64:128], w2k_d, writes=[B_c])
            kb.dma(qg, w2v[:], w2v_d, writes=[B_c])
            kb.dma(qg, peT[0][:], pek_d, writes=[B_c])
            kb.dma(qg, peT[1][:], pev_d, writes=[B_c])
            cst = kb.sb(st, "cst", [128, 2], F32)
            u_ = kb.sb(st, "cu", [128, 128], F32)
            t_ = kb.sb(st, "ct", [128, 128], F32)
            ha = kb.sb(st, "cha", [128, 128], BF16)
            B_cc, B_cw = Buf("kcc"), Buf("cwork")
            for g in range(2):
                for v in range(2):
                    kb.memset(pool, kcc[g][v][:], 0.0, writes=[B_cc])
                kb.memset(pool, vcc[g][:], 0.0, writes=[B_cc])
            for i in range(2):
                for l in range(32):
                    kb.mm(psA[2][:, i:i + 1], w1[i][:, l, :], peT[i][:, l:l + 1], start=(l == 0), stop=(l == 31),
                          reads=[B_c], writes=[B_psA[2]])
            kb.cp(dve, cst[:], psA[2][:, 0:2], reads=[B_psA[2]], writes=[B_cw])
            for g in range(2):
                for i in range(2):
                    src = (kcT if i == 0 else vcT)[g]
                    sv = src[:, :].rearrange("p (n s) -> p n s", s=16)
                    ph = psA[i]
                    for l in range(32):
                        rhs = sv[:, 0:127, l] if l < 16 else sv[:, 1:128, l - 16]
                        kb.mm(ph[:, 0:127], w1[i][:, l, :], rhs, start=(l == 0), stop=(l == 31),
                              reads=[B_c] + B_in, writes=[B_psA[i]])
                    kb.ts(dve, u_[:, 0:127], ph[:, 0:127], cst[:, i:i + 1], None, ALU.add, reads=[B_psA[i], B_cw], writes=[B_cw])
                    kb.tt(dve, t_[:, 0:127], u_[:, 0:127], u_[:, 0:127], ALU.mult, reads=[B_cw], writes=[B_cw])
                    kb.ts(dve, t_[:, 0:127], t_[:, 0:127], 0.044715, 1.0, ALU.mult, ALU.add, reads=[B_cw], writes=[B_cw])
                    kb.tt(dve, t_[:, 0:127], t_[:, 0:127], u_[:, 0:127], ALU.mult, reads=[B_cw], writes=[B_cw])
                    kb.actf(t_[:, 0:127], t_[:, 0:127], AF.Sigmoid, scale=1.5957691216057308, reads=[B_cw], writes=[B_cw])
                    kb.tt(dve, ha[:, 0:127], t_[:, 0:127], u_[:, 0:127], ALU.mult, reads=[B_cw], writes=[B_cw])
                    if i == 0:
                        kb.mm(psA[3][:, 0:127], w2kd[:], ha[:, 0:127], start=True, stop=True, reads=[B_c, B_cw], writes=[B_psA[3]])
                        kb.cp(dve, kcc[g][0][0:64, 0:127], psA[3][0:64, 0:127], reads=[B_psA[3]], writes=[B_cc])
                        kb.cp(dve, kcc[g][1][64:128, 0:127], psA[3][64:128, 0:127], reads=[B_psA[3]], writes=[B_cc])
                    else:
                        kb.mm(psA[3][0:127, 0:64], ha[:, 0:127], w2v[:], start=True, stop=True, reads=[B_c, B_cw], writes=[B_psA[3]])
                        kb.cp(dve, vcc[g][0:127, :], psA[3][0:127, 0:64], reads=[B_psA[3]], writes=[B_cc])
            tap("kcc00", kcc[0][0][:], [128, 128], BF16, reads=[B_cc])
            tap("vcc1", vcc[1][:], [128, 64], BF16, reads=[B_cc])
            kb.barrier()
            st.close()
            st = st_outer

            e_sb = kb.sb(st, "e_sb", [128, 512], F32)
            em = kb.sb(st, "em", [128, 512], F32)
            rs = kb.sb(st, "rs", [128, 512], F32)
            p_f = kb.sb(st, "p_f", [128, 512], F32)
            p_b = kb.sb(st, "p_b", [128, 512], BF16)
            sadj = kb.sb(st, "sadj", [128, 32], F32)
            top8 = kb.sb(st, "top8", [128, 8], F32)
            sel = kb.sb(st, "sel", [128, 32], F32)
            nsel = kb.sb(st, "nsel", [128, 32], BF16)
            nselT = [kb.sb(st, "nselT%d" % i, [128, 128], BF16) for i in range(2)]
            pT = [kb.sb(st, "pT%d" % i, [128, 512], BF16) for i in range(3)]
            acc = [kb.sb(st, "acc%d" % i, [128, 512], F32) for i in range(2)]
            gsc = kb.sb(st, "gsc", [128, 4], F32)
            onb = kb.sb(st, "onb", [128, 512], BF16)
            B_e, B_p, B_sel, B_gsc, B_onb = Buf("e"), Buf("p"), Buf("sel"), Buf("gsc"), Buf("onb")
            B_nselT = [Buf("nselT0"), Buf("nselT1")]
            B_pT = [Buf("pT0"), Buf("pT1"), Buf("pT2")]
            B_acc = [Buf("acc0"), Buf("acc1")]
            for i in range(2):
                kb.memset(pool, nselT[i][:], 0.0, writes=[B_nselT[i]])
            kb.memset(pool, sel[:], 0.0, writes=[B_sel])
            sel_dbg = None
            if "sel" in taps:
                sel_dbg = kb.sb(st, "sel_dbg", [128, NT, 2, 32], F32)
            score_dbg = None
            if "score" in taps:
                score_dbg = kb.sb(st, "score_dbg", [128, NT, 2, 32], F32)
            npt = [0]
            nps = [0]

            def attn_branch(qt, g, kts, kT2, vaug, br, ac, Bac, nsT, BnsT, po, Bpo):
                pv_jobs = []

                def emit_pv(job):
                    idx, kt, pi = job
                    for hh in range(4):
                        kb.mm(po[:, hh * 65:(hh + 1) * 65], pT[pi][:, hh * 128:(hh + 1) * 128], vaug[:, kt, g, :],
                              start=(idx == 0 and hh == 0), stop=(idx == len(kts) - 1 and hh == 3),
                              reads=[B_pT[pi]] + B_in, writes=[Bpo])

                for idx, (kt, mk) in enumerate(kts):
                    si = nps[0] % 2
                    nps[0] += 1
                    ps_ = psA[si]
                    Bps = B_psA[si]
                    ps4 = ps_[:].rearrange("p (h q) -> p h q", h=4)
                    started = False
                    if nsT is not None:
                        kb.mm(ps4, expand[:, kt, :], nsT[:, :].unsqueeze(1).to_broadcast([128, 4, 128]),
                              start=True, stop=False, reads=[B_c, BnsT], writes=[Bps])
                        started = True
                    if mk is not None:
                        kb.mm(ps4, ident_b[:], mk[:, :].unsqueeze(1).to_broadcast([128, 4, 128]),
                              start=not started, stop=False, reads=[B_c, B_const], writes=[Bps])
                        started = True
                    kb.mm(ps4, AL[:, g, qt - kt, :], hind[:], start=not started, stop=False, reads=[B_c], writes=[Bps])
                    started = True
                    for hh in range(4):
                        h = 4 * g + hh
                        kb.mm(ps_[:, hh * 128:(hh + 1) * 128], kT2[g][h % 2][:, kt * 128:(kt + 1) * 128],
                              qT[:, h // 2, qt * 128:(qt + 1) * 128], start=not started, stop=(hh == 3),
                              reads=B_in, writes=[Bps])
                        started = True
                    pi = npt[0] % 3
                    npt[0] += 1
                    kb.actf(pT[pi][:], ps_[:], AF.Exp, reads=[Bps], writes=[B_pT[pi]])
                    if pv_jobs:
                        emit_pv(pv_jobs.pop(0))
                    pv_jobs.append((idx, kt, pi))
                while pv_jobs:
                    emit_pv(pv_jobs.pop(0))
                po3 = po[:, 0:260].rearrange("p (h c) -> p h c", c=65)
                kb.op(dve, lambda: nc.vector.reciprocal(gsc[:], po3[:, :, 64]), reads=[Bpo], writes=[B_gsc])
                kb.tt(dve, gsc[:], gsc[:], sig_nsa[:, qt, br * 8 + g * 4: br * 8 + g * 4 + 4], ALU.mult,
                      reads=[B_gsc] + B_in, writes=[B_gsc])
                for hh in range(4):
                    h = 4 * g + hh
                    kb.stt(dve, ac[:, h * 64:(h + 1) * 64], po[:, hh * 65: hh * 65 + 64], gsc[:, hh:hh + 1],
                           ac[:, h * 64:(h + 1) * 64], ALU.mult, ALU.add, reads=[Bpo, B_gsc], writes=[Bac])

            for qt in range(NT):
                ac = acc[qt % 2]
                Bac = B_acc[qt % 2]
                qs_ = slice(qt * 128, (qt + 1) * 128)
                for g in range(2):
                    si = nps[0] % 2
                    nps[0] += 1
                    ps_ = psA[si]
                    Bps = B_psA[si]
                    for hh in range(4):
                        h = 4 * g + hh
                        kb.mm(ps_[0:127, hh * 128:(hh + 1) * 128], kcc[g][h % 2][:, 0:127], qT[:, h // 2, qs_],
                              start=(hh == 0), stop=(hh == 3), reads=[B_cc] + B_in, writes=[Bps])
                    for hh in range(4):
                        h = 4 * g + hh
                        kb.actf(e_sb[0:127, hh * 128:(hh + 1) * 128], ps_[0:127, hh * 128:(hh + 1) * 128], AF.Exp,
                                bias=cbias[0:127, h, qt:qt + 1], reads=[Bps, B_c], writes=[B_e])
                    kb.tt(dve, em[0:127, :].rearrange("p (h q) -> p h q", h=4),
                          e_sb[0:127, :].rearrange("p (h q) -> p h q", h=4),
                          cmask[0:127, qt, :].unsqueeze(1).to_broadcast([127, 4, 128]), ALU.mult,
                          reads=[B_e, B_c], writes=[B_e])
                    kb.mm(psA[2][0:127, :], ones_f[0:127, 0:127], em[0:127, :], start=True, stop=True,
                          reads=[B_e, B_c], writes=[B_psA[2]])
                    kb.ts(dve, rs[0:127, :], psA[2][0:127, :], 1e-30, None, ALU.max, reads=[B_psA[2]], writes=[B_p])
                    kb.op(dve, lambda: nc.vector.reciprocal(rs[0:127, :], rs[0:127, :]), reads=[B_p], writes=[B_p])
                    kb.tt(dve, p_f[0:127, :], em[0:127, :], rs[0:127, :], ALU.mult, reads=[B_e, B_p], writes=[B_p])
                    kb.cp(pool, p_b[0:127, :], p_f[0:127, :], reads=[B_p], writes=[B_p])
                    pc = psA[3]
                    for hh in range(4):
                        kb.mm(pc[:, 256:288], p_f[0:127, hh * 128:(hh + 1) * 128], ov[0:127, :], start=(hh == 0), stop=False,
                              reads=[B_p, B_c], writes=[B_psA[3]])
                    for hh in range(4):
                        kb.mm(pc[:, hh * 64:(hh + 1) * 64], p_b[0:127, hh * 128:(hh + 1) * 128], vcc[g][0:127, :],
                              start=False, stop=(hh == 3), reads=[B_p, B_cc], writes=[B_psA[3]])
                    for hh in range(4):
                        h = 4 * g + hh
                        kb.ts(dve, ac[:, h * 64:(h + 1) * 64], pc[:, hh * 64:(hh + 1) * 64],
                              sig_nsa[:, qt, g * 4 + hh: g * 4 + hh + 1], None, ALU.mult,
                              reads=[B_psA[3]] + B_in, writes=[Bac])
                    kb.tt(dve, sadj[:], pc[:, 256:288], fadj[:, qt, :], ALU.add, reads=[B_psA[3], B_c], writes=[B_sel])
                    if score_dbg is not None:
                        kb.cp(dve, score_dbg[:, qt, g, :], pc[:, 256:288], reads=[B_psA[3]], writes=[B_sel])
                    kb.op(dve, lambda: nc.vector.max(out=top8[:], in_=sadj[:]), reads=[B_sel], writes=[B_sel])
                    kb.stt(dve, sel[:], sadj[:], top8[:, 7:8], notfut[:, qt, :], ALU.is_ge, ALU.mult,
                           reads=[B_sel, B_c], writes=[B_sel])
                    if sel_dbg is not None:
                        kb.cp(dve, sel_dbg[:, qt, g, :], sel[:], reads=[B_sel], writes=[B_sel])
                    kb.ts(dve, nsel[:], sel[:], 1.0, -NEG, ALU.subtract, ALU.mult, reads=[B_sel], writes=[B_sel])
                    ni = (2 * qt + g) % 2
                    kb.tr(psT[0][0:32, 0:128], nsel[:], ident_b[:], reads=[B_sel, B_const], writes=[B_psT[0]])
                    kb.cp(dve, nselT[ni][0:32, :], psT[0][0:32, 0:128], reads=[B_psT[0]], writes=[B_nselT[ni]])
                    kts = [(kt, trineg if kt == qt else None) for kt in range(qt + 1)]
                    attn_branch(qt, g, kts, ksT, vaug_s, 1, ac, Bac, nselT[ni], B_nselT[ni], psA[4], B_psA[4])
                    kts = []
                    for kt in (qt - 2, qt - 1, qt):
                        if kt < 0:
                            continue
                        kts.append((kt, trineg if kt == qt else (trineg2 if kt == qt - 2 else None)))
                    attn_branch(qt, g, kts, kwT, vaug_w, 2, ac, Bac, None, None, psA[5], B_psA[5])
                kb.cp(pool, onb[:], ac[:], reads=[Bac], writes=[B_onb])
                for c in range(4):
                    kb.tr(psT[1][:, c * 128:(c + 1) * 128], onb[:, c * 128:(c + 1) * 128], ident_b[:],
                          reads=[B_onb, B_const], writes=[B_psT[1]])
                kb.cp(dve, onsaT[:, :, qs_], psT[1][:, 0:512].rearrange("p (c q) -> p c q", c=4),
                      reads=[B_psT[1]], writes=[B_onsaT])
            if sel_dbg is not None:
                tap("sel", sel_dbg[:], [128, NT, 2, 32], F32, reads=[B_sel])
            if score_dbg is not None:
                tap("score", score_dbg[:], [128, NT, 2, 32], F32, reads=[B_sel])
            tap("onsaT", onsaT[:], [128, 4, S], BF16, reads=[B_onsaT])
            kb.barrier()

    convw_d = kb.dram_in("convwT", [128, 4, 4])
    convb_d = kb.dram_in("convbT", [128, 4])
    wq_d = kb.dram_in("ml_wq", [4, 128, 128])
    wk_d = kb.dram_in("ml_wk", [4, 128, 128])
    wv_d = kb.dram_in("ml_wv", [4, 128, 128])
    ngbc_d = kb.dram_in("ng_bc", [128, 512])
    LN_EPS = 1e-5

    def mlstm_phase(b, hT, bfmT, btm, fb_bc, ymlT, B_ymlT, B_b):
        with contextlib.ExitStack() as st:
            xmT = kb.sb(st, "xmT", [128, 4, S + 4], BF16)
            sig_o = kb.sb(st, "sig_o", [128, NT, 512], BF16)
            ifp = kb.sb(st, "ifp", [128, NT, 8], F32)
            B_xm, B_so, B_if = Buf("xm"), Buf("so"), Buf("if")
            kb.memset(pool, xmT[:, :, 0:3], 0.0, writes=[B_xm])
            with contextlib.ExitStack() as st2:
                wfm = kb.sb(st2, "wfmB", [128, KC, 4 * 128], BF16)
                wtm = kb.sb(st2, "wtmB", [128, KC, 520], BF16)
                B_w = Buf("wB")
                wfm_v = wfm_d.rearrange("(k p) n -> p k n", p=128)
                wtm_v = wtm_d.rearrange("(k p) n -> p k n", p=128)
                kb.dma(qg, wfm[:], wfm_v[:, :, 10 * 128:14 * 128], writes=[B_w])
                kb.dma(qg, wtm[:], wtm_v[:, :, 280:800], writes=[B_w])
                n = 0
                for j in range(4):
                    for tg in range(4):
                        pi = n % 4
                        n += 1
                        pb = psA[pi]
                        cs = slice(tg * 512, (tg + 1) * 512)
                        for k in range(KC):
                            kb.mm(pb[:], wfm[:, k, j * 128:(j + 1) * 128], hT[:, k, cs], start=(k == 0), stop=(k == KC - 1),
                                  reads=[B_w, B_hT], writes=[B_psA[pi]])
                        evac(xmT[:, j, 3 + tg * 512: 3 + (tg + 1) * 512], pb[:], bfmT[:, 10 + j:11 + j],
                             reads=[B_psA[pi], B_b], writes=[B_xm])
                for i in range(NT):
                    ts_ = slice(i * 128, (i + 1) * 128)
                    pi = 4 + i % 2
                    pa = psA[pi]
                    for k in range(KC):
                        kb.mm(pa[:], hT[:, k, ts_], wtm[:, k, 8:520], start=(k == 0), stop=False,
                              reads=[B_w, B_hT], writes=[B_psA[pi]])
                    kb.mm(pa[:], ones_r[:], btm[:, 288:800], start=False, stop=True, reads=[B_b, B_const], writes=[B_psA[pi]])
                    kb.actf(sig_o[:, i, :], pa[:], AF.Sigmoid, reads=[B_psA[pi]], writes=[B_so])
                    pc = psA[0 + i % 2]
                    for k in range(KC):
                        kb.mm(pc[:, 0:8], hT[:, k, ts_], wtm[:, k, 0:8], start=(k == 0), stop=False,
                              reads=[B_w, B_hT], writes=[B_psA[i % 2]])
                    kb.mm(pc[:, 0:8], ones_r[:], btm[:, 280:288], start=False, stop=True, reads=[B_b, B_const], writes=[B_psA[i % 2]])
                    kb.cp(dve, ifp[:, i, :], pc[:, 0:8], reads=[B_psA[i % 2]], writes=[B_if])
                kb.tt(dve, ifp[:, :, 4:8], ifp[:, :, 4:8], fb_bc[:, :].unsqueeze(1).to_broadcast([128, NT, 4]), ALU.add,
                      reads=[B_if, B_b], writes=[B_if])
                tap("xmT", xmT[:], [128, 4, S + 4], BF16, reads=[B_xm])
                tap("ifp", ifp[:], [128, NT, 8], F32, reads=[B_if])
                kb.barrier()
            B_c = Buf("mlconst")
            convw = kb.sb(st, "convw", [128, 4, 4], F32)
            convb = kb.sb(st, "convb", [128, 4], F32)
            wq = kb.sb(st, "wq", [128, 4, 128], BF16)
            wk = kb.sb(st, "wk", [128, 4, 128], BF16)
            wv = kb.sb(st, "wv", [128, 4, 128], BF16)
            ng_bc = kb.sb(st, "ng_bc", [128, 512], F32)
            tri_u = kb.sb(st, "tri_u", [128, 128], F32)
            trineg = kb.sb(st, "trinegm", [128, 128], BF16)
            ones_f = kb.sb(st, "ones_fm", [128, 128], F32)
            kb.dma(qs, convw[:], convw_d, writes=[B_c])
            kb.dma(qs, convb[:], convb_d, writes=[B_c])
            kb.dma(qs, ng_bc[:], ngbc_d, writes=[B_c])
            kb.dma(qs, tri_u[:], cdram["tri_u"], writes=[B_c])
            kb.dma(qg, trineg[:], cdram["trineg"], writes=[B_c])
            for t_, d_ in ((wq, wq_d), (wk, wk_d), (wv, wv_d)):
                kb.dma(qg, t_[:], d_.rearrange("h d e -> d h e"), writes=[B_c])
            kb.memset(dve, ones_f[:], 1.0, writes=[B_c])
            lf = kb.sb(st, "lf", [128, NT, 4], F32)
            F_tm = kb.sb(st, "F_tm", [128, NT, 4], F32)
            FL_b = kb.sb(st, "FL_b", [128, NT, 4], F32)
            bcol = kb.sb(st, "bcol", [128, NT, 4], F32)
            wi = kb.sb(st, "wi", [128, NT, 4], F32)
            gst = kb.sb(st, "gst", [128, NT, 4], F32)
            dec = kb.sb(st, "dec", [128, NT, 4], F32)
            B_g = Buf("gates")
            kb.actf(lf[:], ifp[:, :, 4:8], AF.Exp, scale=-1.0, reads=[B_if], writes=[B_g])
            kb.actf(lf[:], lf[:], AF.Ln, bias=1.0, reads=[B_g], writes=[B_g])
            kb.ts(dve, lf[:], lf[:], -1.0, None, ALU.mult, reads=[B_g], writes=[B_g])
            pF = psA[0]
            pL = psA[1]
            for i in range(NT):
                kb.mm(pF[:, i * 4:(i + 1) * 4], tri_u[:], lf[:, i, :], start=True, stop=True, reads=[B_c, B_g], writes=[B_psA[0]])
            for i in range(NT):
                kb.mm(pL[:, i * 4:(i + 1) * 4], ones_f[:], lf[:, i, :], start=True, stop=True, reads=[B_c, B_g], writes=[B_psA[1]])
            kb.cp(dve, F_tm[:], pF[:, 0:64].rearrange("p (i h) -> p i h", h=4), reads=[B_psA[0]], writes=[B_g])
            kb.cp(dve, FL_b[:], pL[:, 0:64].rearrange("p (i h) -> p i h", h=4), reads=[B_psA[1]], writes=[B_g])
            kb.tt(dve, bcol[:], ifp[:, :, 0:4], F_tm[:], ALU.subtract, reads=[B_if, B_g], writes=[B_g])
            kb.actf(wi[:], F_tm[:], AF.Exp, reads=[B_g], writes=[B_g])
            kb.tt(dve, gst[:], FL_b[:], bcol[:], ALU.add, reads=[B_g], writes=[B_g])
            kb.actf(gst[:], gst[:], AF.Exp, reads=[B_g], writes=[B_g])
            kb.actf(dec[:], FL_b[:], AF.Exp, reads=[B_g], writes=[B_g])
            tmpc = kb.sb(st, "tmpc", [128, S], F32)
            xcT = kb.sb(st, "xcT", [128, S], BF16)
            qmT = kb.sb(st, "qmT", [128, S], BF16)
            kmT = kb.sb(st, "kmT", [128, S], BF16)
            k_tm = kb.sb(st, "k_tm", [128, NT, 128], BF16)
            vaug = kb.sb(st, "vaug_m", [128, NT, 129], BF16)
            yml = kb.sb(st, "yml", [128, NT, 512], BF16)
            lfbc = [kb.sb(st, "lfbc%d" % i, [128, 128], F32) for i in range(2)]
            DT = [kb.sb(st, "DT%d" % i, [128, 128], F32) for i in range(2)]
            AT = [kb.sb(st, "AT%d" % i, [128, 128], BF16) for i in range(2)]
            p2s = [kb.sb(st, "p2s%d" % i, [128, 129], F32) for i in range(2)]
            nd = [kb.sb(st, "nd%d" % i, [128, 129], F32) for i in range(2)]
            hout = [kb.sb(st, "hout%d" % i, [128, 128], F32) for i in range(2)]
            kg = [kb.sb(st, "kg%d" % i, [128, 128], BF16) for i in range(2)]
            C_f = kb.sb(st, "C_f", [128, 129], F32)
            C_b = [kb.sb(st, "C_b%d" % i, [128, 129], BF16) for i in range(2)]
            sm = [kb.sb(st, "sm%d" % i, [128, 16], F32) for i in range(2)]
            B_tmpc, B_xc, B_qk, B_ktm, B_va, B_yml, B_Cf = Buf("tmpc"), Buf("xc"), Buf("qk"), Buf("ktm"), Buf("va"), Buf("yml"), Buf("Cf")
            B_w2 = [{nm: Buf(nm + str(i)) for nm in ("lfbc", "DT", "AT", "p2s", "nd", "hout", "kg", "Cb", "sm")} for i in range(2)]
            kb.memset(pool, vaug[:, :, 128:129], 1.0, writes=[B_va])
            SC = float(128 ** -0.5)
            for h in range(4):
                kb.ts(dve, tmpc[:], xmT[:, h, 0:S], convw[:, h, 0:1], None, ALU.mult, reads=[B_xm, B_c], writes=[B_tmpc])
                for k in range(1, 4):
                    kb.stt(dve, tmpc[:], xmT[:, h, k:k + S], convw[:, h, k:k + 1], tmpc[:], ALU.mult, ALU.add,
                           reads=[B_xm, B_c, B_tmpc], writes=[B_tmpc])
                kb.actf(xcT[:], tmpc[:], AF.Silu, bias=convb[:, h:h + 1], reads=[B_tmpc, B_c], writes=[B_xc])
                for tg in range(4):
                    cs = slice(tg * 512, (tg + 1) * 512)
                    kb.mm(psA[2][:], wq[:, h, :], xcT[:, cs], start=True, stop=True, reads=[B_c, B_xc], writes=[B_psA[2]])
                    kb.cp(dve, qmT[:, cs], psA[2][:], reads=[B_psA[2]], writes=[B_qk])
                    kb.mm(psA[3][:], wk[:, h, :], xcT[:, cs], start=True, stop=True, reads=[B_c, B_xc], writes=[B_psA[3]])
                    kb.actf(kmT[:, cs], psA[3][:], AF.Copy, scale=SC, reads=[B_psA[3]], writes=[B_qk])
                    for t4 in range(4):
                        i = tg * 4 + t4
                        kb.mm(psA[4][:, t4 * 128:(t4 + 1) * 128], xcT[:, i * 128:(i + 1) * 128], wk[:, h, :],
                              start=(t4 == 0), stop=(t4 == 3), reads=[B_c, B_xc], writes=[B_psA[4]])
                        kb.mm(psA[5][:, t4 * 128:(t4 + 1) * 128], xmT[:, h, 3 + i * 128: 3 + (i + 1) * 128], wv[:, h, :],
                              start=(t4 == 0), stop=(t4 == 3), reads=[B_c, B_xm], writes=[B_psA[5]])
                    kb.actf(k_tm[:, tg * 4:(tg + 1) * 4, :], psA[4][:].rearrange("p (t d) -> p t d", t=4), AF.Copy, scale=SC,
                            reads=[B_psA[4]], writes=[B_ktm])
                    kb.cp(dve, vaug[:, tg * 4:(tg + 1) * 4, 0:128], psA[5][:].rearrange("p (t d) -> p t d", t=4),
                          reads=[B_psA[5]], writes=[B_va])
                for i in range(NT):
                    w = i % 2
                    Bw = B_w2[w]
                    ts_ = slice(i * 128, (i + 1) * 128)
                    kb.cp(pool, lfbc[w][:], lf[:, i, h:h + 1].to_broadcast([128, 128]), reads=[B_g], writes=[Bw["lfbc"]])
                    pd = psA[0 + w]
                    kb.mm(pd[:, 0:128], ident_b[:], trineg[:], start=True, stop=False, reads=[B_c, B_const], writes=[B_psA[w]])
                    kb.mm(pd[:, 0:128], lfbc[w][:], tri_u[:], start=False, stop=True, reads=[Bw["lfbc"], B_c], writes=[B_psA[w]])
                    kb.actf(DT[w][:], pd[:, 0:128], AF.Exp, bias=bcol[:, i, h:h + 1], reads=[B_psA[w], B_g], writes=[Bw["DT"]])
                    pq = psA[2 + w]
                    kb.mm(pq[:, 0:128], kmT[:, ts_], qmT[:, ts_], start=True, stop=True, reads=[B_qk], writes=[B_psA[2 + w]])
                    kb.tt(dve, AT[w][:], pq[:, 0:128], DT[w][:], ALU.mult, reads=[B_psA[2 + w], Bw["DT"]], writes=[Bw["AT"]])
                    p2 = psA[4]
                    kb.mm(p2[:, 0:129], AT[w][:], vaug[:, i, :], start=True, stop=True, reads=[Bw["AT"], B_va], writes=[B_psA[4]])
                    if i == 0:
                        kb.cp(dve, nd[w][:], p2[:, 0:129], reads=[B_psA[4]], writes=[Bw["nd"]])
                    else:
                        kb.actf(p2s[w][:], p2[:, 0:129], AF.Copy, reads=[B_psA[4]], writes=[Bw["p2s"]])
                        p1 = psA[5]
                        cb_ = C_b[(i - 1) % 2]
                        kb.mm(p1[:, 0:129], qmT[:, ts_], cb_[:], start=True, stop=True,
                              reads=[B_qk, B_w2[(i - 1) % 2]["Cb"]], writes=[B_psA[5]])
                        kb.stt(dve, nd[w][:], p1[:, 0:129], wi[:, i, h:h + 1], p2s[w][:], ALU.mult, ALU.add,
                               reads=[B_psA[5], B_g, Bw["p2s"]], writes=[Bw["nd"]])
                    kb.ts(dve, sm[w][:, 0:1], nd[w][:, 128:129], -1.0, 1.0, ALU.mult, ALU.max, reads=[Bw["nd"]], writes=[Bw["sm"]])
                    kb.ts(dve, sm[w][:, 1:2], nd[w][:, 128:129], 1.0, None, ALU.max, reads=[Bw["nd"]], writes=[Bw["sm"]])
                    kb.tt(dve, sm[w][:, 0:1], sm[w][:, 0:1], sm[w][:, 1:2], ALU.max, reads=[Bw["sm"]], writes=[Bw["sm"]])
                    kb.op(dve, lambda: nc.vector.reciprocal(sm[w][:, 0:1], sm[w][:, 0:1]), reads=[Bw["sm"]], writes=[Bw["sm"]])
                    kb.ts(dve, hout[w][:], nd[w][:, 0:128], sm[w][:, 0:1], None, ALU.mult, reads=[Bw["nd"], Bw["sm"]], writes=[Bw["hout"]])
                    kb.op(dve, lambda: nc.vector.bn_stats(sm[w][:, 2:8], hout[w][:]), reads=[Bw["hout"]], writes=[Bw["sm"]])
                    kb.op(dve, lambda: nc.vector.bn_aggr(sm[w][:, 8:10], sm[w][:, 2:8]), reads=[Bw["sm"]], writes=[Bw["sm"]])
                    kb.ts(dve, sm[w][:, 10:11], sm[w][:, 9:10], LN_EPS, None, ALU.add, reads=[Bw["sm"]], writes=[Bw["sm"]])
                    kb.actf(sm[w][:, 10:11], sm[w][:, 10:11], AF.Sqrt, reads=[Bw["sm"]], writes=[Bw["sm"]])
                    kb.op(dve, lambda: nc.vector.reciprocal(sm[w][:, 10:11], sm[w][:, 10:11]), reads=[Bw["sm"]], writes=[Bw["sm"]])
                    kb.ts(dve, hout[w][:], hout[w][:], sm[w][:, 8:9], sm[w][:, 10:11], ALU.subtract, ALU.mult,
                          reads=[Bw["hout"], Bw["sm"]], writes=[Bw["hout"]])
                    kb.tt(dve, hout[w][:], hout[w][:], ng_bc[:, h * 128:(h + 1) * 128], ALU.mult, reads=[Bw["hout"], B_c], writes=[Bw["hout"]])
                    kb.tt(dve, yml[:, i, h * 128:(h + 1) * 128], hout[w][:], sig_o[:, i, h * 128:(h + 1) * 128], ALU.mult,
                          reads=[Bw["hout"], B_so], writes=[B_yml])
                    if i < NT - 1:
                        kb.ts(pool, kg[w][:], k_tm[:, i, :], gst[:, i, h:h + 1], None, ALU.mult, reads=[B_ktm, B_g], writes=[Bw["kg"]])
                        pcx = psA[5] if i == 0 else psA[4]
                        Bpc = B_psA[5] if i == 0 else B_psA[4]
                        kb.mm(pcx[:, 256:385], kg[w][:], vaug[:, i, :], start=True, stop=True, reads=[Bw["kg"], B_va], writes=[Bpc])
                        if i == 0:
                            kb.cp(dve, C_f[:], pcx[:, 256:385], reads=[Bpc], writes=[B_Cf])
                        else:
                            kb.stt(dve, C_f[:], C_f[:], dec[:, i, h:h + 1], pcx[:, 256:385], ALU.mult, ALU.add,
                                   reads=[B_Cf, B_g, Bpc], writes=[B_Cf])
                        kb.actf(C_b[w][:], C_f[:], AF.Copy, reads=[B_Cf], writes=[Bw["Cb"]])
            tap("yml", yml[:], [128, NT, 512], BF16, reads=[B_yml])
            for i in range(NT):
                p = i % 2
                for c in range(4):
                    kb.tr(psT[p][:, c * 128:(c + 1) * 128], yml[:, i, c * 128:(c + 1) * 128], ident_b[:],
                          reads=[B_yml, B_const], writes=[B_psT[p]])
                kb.cp(dve, ymlT[:, :, i * 128:(i + 1) * 128], psT[p][:, 0:512].rearrange("p (c q) -> p c q", c=4),
                      reads=[B_psT[p]], writes=[B_ymlT])
            kb.barrier()

    pa_d = kb.dram_in("proj_a", [512, D])
    pb_d = kb.dram_in("proj_b", [512, D])
    wo_d = kb.dram_in("w_out", [D, D])
    if stage >= 6:
        rw_d = kb.dram_in("router_w", [D, 32])
        rb_d = kb.dram_in("router_brow", [128, 32])
        wup_d = kb.dram_in("exp_w_up", [32, D, 2 * D])
        wdn_d = kb.dram_in("exp_w_down", [32, D, D])
        bupT_d = kb.dram_in("b_upT", [128, 32, 16])
        bdn_d = kb.dram_in("exp_b_down", [32, D])
        fg_d = kb.dram_in("fg_bc", [128, D])

    def merge_phase(b, hT, onsaT, ymlT, bfmT, B_b, B_onsaT, B_ymlT):
        with contextlib.ExitStack() as st:
            wmg = kb.sb(st, "wmg", [128, KC, 16 * 128], BF16)
            pa = kb.sb(st, "pa", [128, 4, D], BF16)
            pb_ = kb.sb(st, "pb", [128, 4, D], BF16)
            wo = kb.sb(st, "wo", [128, KC, D], BF16)
            g1 = kb.sb(st, "g1bc", [128, D], F32)
            B_w = Buf("wmerge")
            kb.dma(qg, wmg[:], wfm_d.rearrange("(k p) n -> p k n", p=128)[:, :, 14 * 128:30 * 128], writes=[B_w])
            kb.dma(qg, pa[:], pa_d.rearrange("(k p) n -> p k n", p=128), writes=[B_w])
            kb.dma(qg, pb_[:], pb_d.rearrange("(k p) n -> p k n", p=128), writes=[B_w])
            kb.dma(qg, wo[:], wo_d.rearrange("(k p) n -> p k n", p=128), writes=[B_w])
            kb.dma(qs, g1[:], gate_d[b, 0], reads=[B_gd], writes=[B_w])
            preT = kb.sb(st, "preT", [128, KC, 512], BF16)
            gsb = [kb.sb(st, "gsb%d" % i, [128, 512], BF16) for i in range(2)]
            ta = kb.sb(st, "ta", [128, 512], F32)
            tb = kb.sb(st, "tb", [128, 512], F32)
            xb = [kb.sb(st, "xb4_%d" % i, [128, D], F32) for i in range(2)]
            x1 = [kb.sb(st, "x1_%d" % i, [128, D], F32) for i in range(2)]
            xn = [kb.sb(st, "xn4_%d" % i, [128, D], BF16) for i in range(2)]
            h2s = [kb.sb(st, "h2s%d" % i, [128, KC, 128], BF16) for i in range(2)]
            junk = kb.sb(st, "junk4", [128, D], BF16)
            ss = kb.sb(st, "ss4", [128, NT], F32)
            B_pre, B_ta, B_tb, B_junk, B_ss = Buf("pre"), Buf("ta"), Buf("tb"), Buf("junk4"), Buf("ss4")
            B_gsb = [Buf("gsb0"), Buf("gsb1")]
            B_xb = [Buf("xb0"), Buf("xb1")]
            B_x1 = [Buf("x1_0"), Buf("x1_1")]
            B_xn = [Buf("xn0"), Buf("xn1")]
            B_h2s = [Buf("h2s0"), Buf("h2s1")]
            for tg in range(4):
                cs = slice(tg * 512, (tg + 1) * 512)
                for fc in range(8):
                    for br in range(2):
                        src, Bsrc, pw = (onsaT, B_onsaT, pa) if br == 0 else (ymlT, B_ymlT, pb_)
                        pv = psA[0 + br]
                        for k in range(4):
                            kb.mm(pv[:], pw[:, k, fc * 128:(fc + 1) * 128], src[:, k, cs], start=(k == 0), stop=(k == 3),
                                  reads=[B_w, Bsrc], writes=[B_psA[br]])
                        pg = psA[2 + br]
                        gcol = br * 8 + fc
                        for k in range(KC):
                            kb.mm(pg[:], wmg[:, k, gcol * 128:(gcol + 1) * 128], hT[:, k, cs], start=(k == 0), stop=(k == KC - 1),
                                  reads=[B_w, B_hT], writes=[B_psA[2 + br]])
                        kb.actf(gsb[br][:], pg[:], AF.Sigmoid, bias=bfmT[:, 14 + gcol:15 + gcol],
                                reads=[B_psA[2 + br], B_b], writes=[B_gsb[br]])
                        if br == 0:
                            kb.tt(dve, ta[:], pv[:], gsb[0][:], ALU.mult, reads=[B_psA[0], B_gsb[0]], writes=[B_ta])
                        else:
                            kb.tt(dve, tb[:], pv[:], gsb[1][:], ALU.mult, reads=[B_psA[1], B_gsb[1]], writes=[B_tb])
                    kb.tt(pool, preT[:, fc, :], ta[:], tb[:], ALU.add, reads=[B_ta, B_tb], writes=[B_pre])
                for t4 in range(4):
                    i = tg * 4 + t4
                    p = i % 2
                    kb.dma(qs, xb[p][:], x_d[b, i * 128:(i + 1) * 128, :], writes=[B_xb[p]])
                    for hf in range(2):
                        pm = psA[4 + hf]
                        for k in range(KC):
                            kb.mm(pm[:], preT[:, k, t4 * 128:(t4 + 1) * 128], wo[:, k, hf * 512:(hf + 1) * 512],
                                  start=(k == 0), stop=(k == KC - 1), reads=[B_pre, B_w], writes=[B_psA[4 + hf]])
                        hs = slice(hf * 512, (hf + 1) * 512)
                        kb.tt(dve, x1[p][:, hs], pm[:], g1[:, hs], ALU.mult, reads=[B_psA[4 + hf], B_w], writes=[B_x1[p]])
                        kb.tt(dve, x1[p][:, hs], x1[p][:, hs], xb[p][:, hs], ALU.add, reads=[B_x1[p], B_xb[p]], writes=[B_x1[p]])
                    kb.dma(qs, x1_d[b, i * 128:(i + 1) * 128, :], x1[p][:], reads=[B_x1[p]], writes=[B_x1d])
                    kb.actf(junk[:], x1[p][:], AF.Square, accum_out=ss[:, i:i + 1], reads=[B_x1[p]], writes=[B_junk, B_ss])
                    kb.ts(dve, ss[:, i:i + 1], ss[:, i:i + 1], 1.0 / D, RMS_EPS, ALU.mult, ALU.add, reads=[B_ss], writes=[B_ss])
                    kb.actf(ss[:, i:i + 1], ss[:, i:i + 1], AF.Sqrt, reads=[B_ss], writes=[B_ss])
                    kb.op(dve, lambda: nc.vector.reciprocal(ss[:, i:i + 1], ss[:, i:i + 1]), reads=[B_ss], writes=[B_ss])
                    kb.ts(dve, xn[p][:], x1[p][:], ss[:, i:i + 1], None, ALU.mult, reads=[B_ss, B_x1[p]], writes=[B_xn[p]])
                    for c in range(KC):
                        kb.tr(psT[p][:, c * 128:(c + 1) * 128], xn[p][:, c * 128:(c + 1) * 128], ident_b[:],
                              reads=[B_xn[p], B_const], writes=[B_psT[p]])
                    for c in range(KC):
                        kb.actf(h2s[p][:, c, :], psT[p][:, c * 128:(c + 1) * 128], AF.Identity,
                                scale=s2T[:, c, b:b + 1], bias=modT[:, 24 + c, b:b + 1],
                                reads=[B_psT[p], B_mod], writes=[B_h2s[p]])
                    kb.dma(qs, h2T_d[b, :, :, i * 128:(i + 1) * 128], h2s[p][:], reads=[B_h2s[p]], writes=[B_h2d])
            kb.barrier()

    def moe_phase(b, nexp):
        with contextlib.ExitStack() as st:
            h2T = kb.sb(st, "h2T", [128, KC, S], BF16)
            acc = kb.sb(st, "acc", [128, NT, D], F32)
            gatew = kb.sb(st, "gatew", [128, NT, 32], F32)
            rw = kb.sb(st, "rw", [128, KC, 32], BF16)
            rb = kb.sb(st, "rb", [128, 32], BF16)
            bupT = kb.sb(st, "bupT", [128, 32, 16], F32)
            B_h2, B_acc, B_gw, B_c = Buf("h2T"), Buf("acc"), Buf("gatew"), Buf("moeconst")
            kb.dma(qs, h2T[:], h2T_d[b], reads=[B_h2d], writes=[B_h2])
            kb.dma(qg, rw[:], rw_d.rearrange("(k p) n -> p k n", p=128), writes=[B_c])
            kb.dma(qg, rb[:], rb_d, writes=[B_c])
            kb.dma(qs, bupT[:], bupT_d, writes=[B_c])
            lg = [kb.sb(st, "lg%d" % i, [128, 32], F32) for i in range(2)]
            ex = [kb.sb(st, "ex%d" % i, [128, 32], F32) for i in range(2)]
            t8 = [kb.sb(st, "t8%d" % i, [128, 12], F32) for i in range(2)]
            B_r = [Buf("r0"), Buf("r1")]
            gwT = kb.sb(st, "gwT", [128, S], BF16)
            bdall = kb.sb(st, "bdall", [128, D], BF16)
            B_gwT, B_bd = Buf("gwT"), Buf("bdall")
            kb.memset(pool, gwT[:], 0.0, writes=[B_gwT])
            kb.memset(pool, bdall[:], 0.0, writes=[B_bd])
            kb.dma(qg, bdall[0:32, :], bdn_d, writes=[B_bd])
            for i in range(NT):
                p = i % 2
                pr = psA[p]
                for k in range(KC):
                    kb.mm(pr[:, 0:32], h2T[:, k, i * 128:(i + 1) * 128], rw[:, k, :], start=(k == 0), stop=False,
                          reads=[B_h2, B_c], writes=[B_psA[p]])
                kb.mm(pr[:, 0:32], ones_r[:], rb[:], start=False, stop=True, reads=[B_c, B_const], writes=[B_psA[p]])
                kb.cp(dve, lg[p][:], pr[:, 0:32], reads=[B_psA[p]], writes=[B_r[p]])
                kb.op(dve, lambda: nc.vector.max(out=t8[p][:, 0:8], in_=lg[p][:]), reads=[B_r[p]], writes=[B_r[p]])
                kb.ts(dve, t8[p][:, 8:9], t8[p][:, 0:1], -1.0, None, ALU.mult, reads=[B_r[p]], writes=[B_r[p]])
                kb.actf(ex[p][:], lg[p][:], AF.Exp, bias=t8[p][:, 8:9], reads=[B_r[p]], writes=[B_r[p]])
                kb.stt(dve, ex[p][:], lg[p][:], t8[p][:, 3:4], ex[p][:], ALU.is_ge, ALU.mult, reads=[B_r[p]], writes=[B_r[p]])
                kb.op(dve, lambda: nc.vector.reduce_sum(t8[p][:, 9:10], ex[p][:], AX.X), reads=[B_r[p]], writes=[B_r[p]])
                kb.op(dve, lambda: nc.vector.reciprocal(t8[p][:, 9:10], t8[p][:, 9:10]), reads=[B_r[p]], writes=[B_r[p]])
                kb.ts(dve, gatew[:, i, :], ex[p][:], t8[p][:, 9:10], None, ALU.mult, reads=[B_r[p]], writes=[B_gw])
                kb.tr(psA[2 + p][0:32, 0:128], gatew[:, i, :], ident_f[:], reads=[B_gw, B_const], writes=[B_psA[2 + p]])
                kb.cp(dve, gwT[0:32, i * 128:(i + 1) * 128], psA[2 + p][0:32, 0:128], reads=[B_psA[2 + p]], writes=[B_gwT])
            tap("gatew", gatew[:], [128, NT, 32], F32, reads=[B_gw])
            aT = kb.sb(st, "aT", [128, KC, S], BF16)
            NRING = 5
            ring = [kb.sb(st, "ring%d" % i, [128, KC, 512], BF16) for i in range(NRING)]
            B_ring = [Buf("ring%d" % i) for i in range(NRING)]
            gc = [kb.sb(st, "gc%d" % i, [128, 512], F32) for i in range(2)]
            sg = [kb.sb(st, "sg%d" % i, [128, 512], F32) for i in range(2)]
            l1 = [kb.sb(st, "l1%d" % i, [128, 512], F32) for i in range(2)]
            B_wk = [{nm: Buf(nm + str(i)) for nm in ("gc", "sg", "l1")} for i in range(2)]
            B_aT = [Buf("aT%d" % i) for i in range(4)]
            pendC = []
            wup_v = wup_d.rearrange("e (k p) n -> e p k n", p=128)
            wdn_v = wdn_d.rearrange("e (k p) n -> e p k n", p=128)
            rn = [0]

            def load_piece(e, pc):
                r = rn[0] % NRING
                rn[0] += 1
                if pc < 4:
                    kb.dma(qg, ring[r][:, :, 0:256], wup_v[e, :, :, pc * 256:(pc + 1) * 256], writes=[B_ring[r]])
                    kb.dma(qg, ring[r][:, :, 256:512], wup_v[e, :, :, D + pc * 256: D + (pc + 1) * 256], writes=[B_ring[r]])
                else:
                    hf = pc - 4
                    kb.dma(qg, ring[r][:], wdn_v[e, :, :, hf * 512:(hf + 1) * 512], writes=[B_ring[r]])
                return r

            pending = []
            order = [(e, pc) for e in range(nexp) for pc in range(6)]
            nxt = [0]

            def prefetch(upto):
                while nxt[0] < len(order) and nxt[0] < upto:
                    e_, pc_ = order[nxt[0]]
                    pending.append(load_piece(e_, pc_))
                    nxt[0] += 1

            prefetch(3)
            na = 0
            for e in range(nexp):
                slots = []
                for qq in range(4):
                    prefetch(e * 6 + qq + 3)
                    r = pending.pop(0)
                    slots.append(r)
                    for tg in range(4):
                        cs = slice(tg * 512, (tg + 1) * 512)
                        for pr_ in range(2):
                            w = na % 2
                            na += 1
                            Bw = B_wk[w]
                            ch = 2 * qq + pr_
                            pg = psA[0 + w]
                            pl = psA[2 + w]
                            for k in range(KC):
                                kb.mm(pg[:], ring[r][:, k, pr_ * 128:(pr_ + 1) * 128], h2T[:, k, cs], start=(k == 0), stop=(k == KC - 1),
                                      reads=[B_ring[r], B_h2], writes=[B_psA[w]])
                            for k in range(KC):
                                kb.mm(pl[:], ring[r][:, k, 256 + pr_ * 128: 256 + (pr_ + 1) * 128], h2T[:, k, cs], start=(k == 0), stop=(k == KC - 1),
                                      reads=[B_ring[r], B_h2], writes=[B_psA[2 + w]])
                            kb.ts(dve, gc[w][:], pg[:], bupT[:, e, ch:ch + 1], 7.0, ALU.add, ALU.min,
                                  reads=[B_psA[w], B_c], writes=[Bw["gc"]])
                            kb.actf(sg[w][:], gc[w][:], AF.Sigmoid, scale=1.702, reads=[Bw["gc"]], writes=[Bw["sg"]])
                            kb.actf(l1[w][:], pl[:], AF.Identity, bias=bupT[:, e, 8 + ch:9 + ch], reads=[B_psA[2 + w], B_c], writes=[Bw["l1"]])
                            kb.ts(pool, l1[w][:], l1[w][:], 7.0, -7.0, ALU.min, ALU.max, reads=[Bw["l1"]], writes=[Bw["l1"]])
                            kb.tt(pool, sg[w][:], sg[w][:], gc[w][:], ALU.mult, reads=[Bw["sg"], Bw["gc"]], writes=[Bw["sg"]])
                            if pendC:
                                pendC.pop(0)()

                            def _fin(ch=ch, cs=cs, w=w, Bw=Bw, tg=tg):
                                kb.stt(dve, aT[:, ch, cs], l1[w][:], 1.0, sg[w][:], ALU.add, ALU.mult,
                                       reads=[Bw["l1"], Bw["sg"]], writes=[B_aT[tg]])
                            pendC.append(_fin)
                while pendC:
                    pendC.pop(0)()
                prefetch(e * 6 + 4 + 3)
                rd0 = pending.pop(0)
                prefetch(e * 6 + 5 + 3)
                rd1 = pending.pop(0)
                rds = [rd0, rd1]
                for i in range(NT):
                    for hf in range(2):
                        w = (i * 2 + hf) % 2
                        pd = psA[4 + w]
                        for k in range(KC):
                            kb.mm(pd[:], aT[:, k, i * 128:(i + 1) * 128], ring[rds[hf]][:, k, :], start=(k == 0), stop=(k == KC - 1),
                                  reads=[B_aT[i // 4], B_ring[rds[hf]]], writes=[B_psA[4 + w]])
                        av = acc[:, i, hf * 512:(hf + 1) * 512]
                        if e == 0:
                            kb.ts(dve, av, pd[:], gatew[:, i, e:e + 1], None, ALU.mult, reads=[B_psA[4 + w], B_gw], writes=[B_acc])
                        else:
                            kb.stt(dve, av, pd[:], gatew[:, i, e:e + 1], av, ALU.mult, ALU.add,
                                   reads=[B_psA[4 + w], B_gw, B_acc], writes=[B_acc])
            for i in range(NT):
                for hf in range(2):
                    w = (i * 2 + hf) % 2
                    pd = psA[4 + w]
                    kb.mm(pd[:], gwT[:, i * 128:(i + 1) * 128], bdall[:, hf * 512:(hf + 1) * 512], start=True, stop=True,
                          reads=[B_gwT, B_bd], writes=[B_psA[4 + w]])
                    av = acc[:, i, hf * 512:(hf + 1) * 512]
                    kb.tt(dve, av, av, pd[:], ALU.add, reads=[B_psA[4 + w], B_acc], writes=[B_acc])
            kb.barrier()
            g2 = ring[0][:].rearrange("p k n -> p (k n)")[:, 0:2 * D].bitcast(F32)
            fg = ring[1][:].rearrange("p k n -> p (k n)")[:, 0:2 * D].bitcast(F32)
            x1t = [ring[2][:].rearrange("p k n -> p (k n)")[:, 0:2 * D].bitcast(F32),
                   ring[3][:].rearrange("p k n -> p (k n)")[:, 0:2 * D].bitcast(F32)]
            ot = [aT[:, 0:2, :].rearrange("p k n -> p (k n)")[:, 0:2 * D].bitcast(F32),
                  aT[:, 2:4, :].rearrange("p k n -> p (k n)")[:, 0:2 * D].bitcast(F32)]
            junk = aT[:, 4, 0:D]
            B_f = Buf("fin")
            B_x1t = [Buf("x1t0"), Buf("x1t1")]
            B_ot = [Buf("ot0"), Buf("ot1")]
            ssf = kb.sb(st, "ssf", [128, NT], F32)
            kb.dma(qs, g2, gate_d[b, 1], reads=[B_gd], writes=[B_f])
            kb.dma(qs, fg, fg_d, writes=[B_f])
            for i in range(NT):
                p = i % 2
                kb.dma(qs, x1t[p], x1_d[b, i * 128:(i + 1) * 128, :], reads=[B_x1d], writes=[B_x1t[p]])
                kb.tt(dve, acc[:, i, :], acc[:, i, :], g2, ALU.mult, reads=[B_acc, B_f], writes=[B_acc])
                kb.tt(dve, x1t[p], x1t[p], acc[:, i, :], ALU.add, reads=[B_x1t[p], B_acc], writes=[B_x1t[p]])
                kb.actf(junk, x1t[p], AF.Square, accum_out=ssf[:, i:i + 1], reads=[B_x1t[p]], writes=[B_f])
                kb.ts(dve, ssf[:, i:i + 1], ssf[:, i:i + 1], 1.0 / D, RMS_EPS, ALU.mult, ALU.add, reads=[B_f], writes=[B_f])
                kb.actf(ssf[:, i:i + 1], ssf[:, i:i + 1], AF.Sqrt, reads=[B_f], writes=[B_f])
                kb.op(dve, lambda: nc.vector.reciprocal(ssf[:, i:i + 1], ssf[:, i:i + 1]), reads=[B_f], writes=[B_f])
                kb.stt(dve, ot[p], x1t[p], ssf[:, i:i + 1], fg, ALU.mult, ALU.mult, reads=[B_x1t[p], B_f], writes=[B_ot[p]])
                kb.dma(qs, out_d[b, i * 128:(i + 1) * 128, :], ot[p], reads=[B_ot[p]], writes=[B_out])
            kb.barrier()

    def mixer(b, sq):
        hT = kb.sb(sq, "hT%d" % b, [128, KC, S], BF16)
        bfmT = kb.sb(sq, "bfmT%d" % b, [128, N_FM], F32)
        btm = kb.sb(sq, "btm%d" % b, [128, TM_W], BF16)
        fb_bc = kb.sb(sq, "fb_bc%d" % b, [128, 4], F32)
        onsaT = kb.sb(sq, "onsaT%d" % b, [128, 4, S], BF16)
        B_onsaT = Buf("onsaT")
        B_ymlT = Buf("ymlT")
        B_b = Buf("bias")
        kb.dma(qs, bfmT[:], bfmT_d, writes=[B_b])
        kb.dma(qg, btm[:], btm_d, writes=[B_b])
        kb.dma(qs, fb_bc[:], fbias_d, writes=[B_b])
        with contextlib.ExitStack() as st:
            xb = [kb.sb(st, "xb%d" % i, [128, D], F32) for i in range(2)]
            xn = [kb.sb(st, "xn%d" % i, [128, D], BF16) for i in range(2)]
            junk = kb.sb(st, "junk", [128, D], BF16)
            ss = kb.sb(st, "ss", [128, NT], F32)
            rstd = kb.sb(st, "rstd", [128, NT], F32)
            B_x = [Buf("x0"), Buf("x1")]
            B_xn = [Buf("xn0"), Buf("xn1")]
            B_st = Buf("stats")
            B_junk = Buf("junk")
            for i in range(NT):
                p = i % 2
                kb.dma(qs, xb[p][:], x_d[b, i * 128:(i + 1) * 128, :], writes=[B_x[p]])
                kb.actf(junk[:], xb[p][:], AF.Square, accum_out=ss[:, i:i + 1], reads=[B_x[p]], writes=[B_junk, B_st])
                kb.ts(dve, rstd[:, i:i + 1], ss[:, i:i + 1], 1.0 / D, RMS_EPS, ALU.mult, ALU.add, reads=[B_st], writes=[B_st])
                kb.actf(rstd[:, i:i + 1], rstd[:, i:i + 1], AF.Sqrt, reads=[B_st], writes=[B_st])
                kb.op(dve, lambda: nc.vector.reciprocal(rstd[:, i:i + 1], rstd[:, i:i + 1]), reads=[B_st], writes=[B_st])
                kb.ts(dve, xn[p][:], xb[p][:], rstd[:, i:i + 1], None, ALU.mult, reads=[B_st, B_x[p]], writes=[B_xn[p]])
                for c in range(KC):
                    kb.tr(psT[p][:, c * 128:(c + 1) * 128], xn[p][:, c * 128:(c + 1) * 128], ident_b[:],
                          reads=[B_xn[p], B_const], writes=[B_psT[p]])
                for c in range(KC):
                    kb.actf(hT[:, c, i * 128:(i + 1) * 128], psT[p][:, c * 128:(c + 1) * 128], AF.Identity,
                            scale=s1T[:, c, b:b + 1], bias=modT[:, c, b:b + 1],
                            reads=[B_psT[p], B_mod], writes=[B_hT])
            tap("hT", hT[:], [128, KC, S], BF16, reads=[B_hT])
            kb.barrier()

        nsa = contextlib.ExitStack()
        qT = kb.sb(nsa, "qT", [128, 4, S], BF16)
        kcT = [kb.sb(nsa, "kcT%d" % g, [128, S], BF16) for g in range(2)]
        vcT = [kb.sb(nsa, "vcT%d" % g, [128, S], BF16) for g in range(2)]
        ksT = [[kb.sb(nsa, "ksT%d%d" % (g, v), [128, S], BF16) for v in range(2)] for g in range(2)]
        kwT = [[kb.sb(nsa, "kwT%d%d" % (g, v), [128, S], BF16) for v in range(2)] for g in range(2)]
        vaug_s = kb.sb(nsa, "vaug_s", [128, NT, 2, 65], BF16)
        vaug_w = kb.sb(nsa, "vaug_w", [128, NT, 2, 65], BF16)
        sig_nsa = kb.sb(nsa, "sig_nsa", [128, NT, 24], F32)
        B_q, B_kc, B_ks, B_kw, B_v, B_sn = Buf("q"), Buf("kc"), Buf("ks"), Buf("kw"), Buf("v"), Buf("sn")
        for g in range(2):
            zr = slice(64, 128) if g == 0 else slice(0, 64)
            kb.memset(pool, kcT[g][zr, :], 0.0, writes=[B_kc])
            kb.memset(pool, vcT[g][zr, :], 0.0, writes=[B_kc])
            for t_, Bt in ((ksT, B_ks), (kwT, B_kw)):
                kb.memset(pool, t_[g][0][64:128, :], 0.0, writes=[Bt])
                kb.memset(pool, t_[g][1][0:64, :], 0.0, writes=[Bt])
        kb.memset(pool, vaug_s[:, :, :, 64:65], 1.0, writes=[B_v])
        kb.memset(pool, vaug_w[:, :, :, 64:65], 1.0, writes=[B_v])
        with contextlib.ExitStack() as st:
            B_pacc = Buf("pacc")
            wfm = kb.sb(st, "wfmA", [128, KC, 10 * 128], BF16)
            wtm = kb.sb(st, "wtmA", [128, KC, 280], BF16)
            B_w = Buf("wA")
            wfm_v = wfm_d.rearrange("(k p) n -> p k n", p=128)
            wtm_v = wtm_d.rearrange("(k p) n -> p k n", p=128)
            kb.dma(qg, wfm[:], wfm_v[:, :, 0:10 * 128], writes=[B_w])
            kb.dma(qg, wtm[:], wtm_v[:, :, 0:280], writes=[B_w])
            n = 0
            for j in range(10):
                for tg in range(4):
                    pi = n % 4
                    n += 1
                    pb = psA[pi]
                    cs = slice(tg * 512, (tg + 1) * 512)
                    for k in range(KC):
                        kb.mm(pb[:], wfm[:, k, j * 128:(j + 1) * 128], hT[:, k, cs], start=(k == 0), stop=(k == KC - 1),
                              reads=[B_w, B_hT], writes=[B_psA[pi]])
                    rd = [B_psA[pi], B_b]
                    if j < 4:
                        evac(qT[:, j, cs], pb[:], bfmT[:, j:j + 1], mul=0.125, reads=rd, writes=[B_q])
                    elif j in (4, 5):
                        tl = kcT if j == 4 else vcT
                        evac(tl[0][0:64, cs], pb[0:64, :], bfmT[0:64, j:j + 1], reads=rd, writes=[B_kc], force_dve=True)
                        evac(tl[1][64:128, cs], pb[64:128, :], bfmT[64:128, j:j + 1], reads=rd, writes=[B_kc], force_dve=True)
                    else:
                        tl, Bt = (ksT, B_ks) if j < 8 else (kwT, B_kw)
                        g = (j - 6) % 2
                        evac(tl[g][0][0:64, cs], pb[0:64, :], bfmT[0:64, j:j + 1], reads=rd, writes=[Bt], force_dve=True)
                        evac(tl[g][1][64:128, cs], pb[64:128, :], bfmT[64:128, j:j + 1], reads=rd, writes=[Bt], force_dve=True)
            for i in range(NT):
                pi = 4 + i % 2
                pa = psA[pi]
                ts_ = slice(i * 128, (i + 1) * 128)
                for k in range(KC):
                    kb.mm(pa[:, 0:280], hT[:, k, ts_], wtm[:, k, :], start=(k == 0), stop=False,
                          reads=[B_w, B_hT], writes=[B_psA[pi]])
                kb.mm(pa[:, 0:280], ones_r[:], btm[:, 0:280], start=False, stop=True, reads=[B_b, B_const], writes=[B_psA[pi]])
                kb.cp(dve, vaug_s[:, i, :, 0:64], pa[:, 0:128].rearrange("p (g d) -> p g d", g=2), reads=[B_psA[pi]], writes=[B_v, B_pacc])
                kb.cp(dve, vaug_w[:, i, :, 0:64], pa[:, 128:256].rearrange("p (g d) -> p g d", g=2), reads=[B_psA[pi]], writes=[B_v, B_pacc])
                kb.actf(sig_nsa[:, i, :], pa[:, 256:280], AF.Sigmoid, reads=[B_psA[pi]], writes=[B_sn, B_pacc])
            tap("qT", qT[:], [128, 4, S], BF16, reads=[B_q])
            tap("ksT00", ksT[0][0][:], [128, S], BF16, reads=[B_ks])
            tap("kwT11", kwT[1][1][:], [128, S], BF16, reads=[B_kw])
            tap("kcT1", kcT[1][:], [128, S], BF16, reads=[B_kc])
            tap("vaug_s", vaug_s[:], [128, NT, 2, 65], BF16, reads=[B_v])
            tap("sig_nsa", sig_nsa[:], [128, NT, 24], F32, reads=[B_sn])
            kb.barrier()
        if stage <= 2:
            nsa.close()
            return
        nsa_phase(b, qT, kcT, vcT, ksT, kwT, vaug_s, vaug_w, sig_nsa, onsaT, B_onsaT,
                  [B_q, B_kc, B_ks, B_kw, B_v, B_sn])
        nsa.close()
        if stage <= 3:
            return
        ymlT = kb.sb(sq, "ymlT%d" % b, [128, 4, S], BF16)
        mlstm_phase(b, hT, bfmT, btm, fb_bc, ymlT, B_ymlT, B_b)
        if stage <= 4:
            return
        merge_phase(b, hT, onsaT, ymlT, bfmT, B_b, B_onsaT, B_ymlT)

    B_out = Buf("out")
    for b in range(NB if stage > 7 else 1):
        with contextlib.ExitStack() as sq:
            mixer(b, sq)
            kb.barrier()
        if stage >= 6:
            moe_phase(b, 32 if stage >= 7 else 2)

    kb.barrier()
    return kb, tap_out


def prep_inputs(inputs):
    f = lambda a: np.ascontiguousarray(np.asarray(a, dtype=np.float32))
    x = f(inputs["x"])
    c = f(inputs["c"])
    w_in = f(inputs["w_in"][0])
    b_in = f(inputs["b_in"][0])
    fc = fm_cols()
    tc_ = tm_cols()
    shared = {
        "ada_w": f(inputs["ada_w"][0]),
        "ada_bT": colT(f(inputs["ada_b"][0]), 48),
        "n1gT": colT(f(inputs["norm1_g"][0]), KC),
        "n2gT": colT(f(inputs["norm2_g"][0]), KC),
        "w_fm": np.ascontiguousarray(w_in[:, fc.reshape(-1)]),
        "b_fmT": np.ascontiguousarray(b_in[fc].T),
        "w_tm": np.ascontiguousarray(w_in[:, tc_]),
    }
    brow = np.zeros((128, 2 * D), np.float32)
    ab = f(inputs["ada_b"][0])
    brow[0, :D] = ab[2 * D:3 * D]
    brow[0, D:] = ab[5 * D:6 * D]
    shared["ada_brow"] = brow
    btm = np.zeros((128, TM_W), np.float32)
    btm[0] = b_in[tc_]
    shared["b_tm"] = btm
    shared["fbias_bc"] = np.ascontiguousarray(np.broadcast_to(f(inputs["ml_f_bias"][0])[None, :], (128, 4)))
    for nm in ("cmp_w1_k", "cmp_w1_v", "cmp_w2_k", "cmp_w2_v"):
        shared[nm] = f(inputs[nm][0])
    for nm, src in (("peT_k", "cmp_pe_k"), ("peT_v", "cmp_pe_v")):
        pt = np.zeros((128, 32), np.float32)
        pt[0:64] = f(inputs[src][0]).T
        pt[64:128] = 0.0
        shared[nm] = pt
    cw = f(inputs["ml_conv_w"][0])
    shared["convwT"] = np.ascontiguousarray(cw.reshape(4, 4, 128).transpose(2, 1, 0))
    shared["convbT"] = colT(f(inputs["ml_conv_b"][0]), 4)
    for nm in ("ml_wq", "ml_wk", "ml_wv"):
        shared[nm] = f(inputs[nm][0])
    shared["ng_bc"] = np.ascontiguousarray(np.broadcast_to(f(inputs["ml_norm_g"][0])[None, :], (128, 512)))
    for nm in ("proj_a", "proj_b", "w_out", "router_w", "exp_w_up", "exp_w_down", "exp_b_down"):
        shared[nm] = f(inputs[nm][0])
    rbr = np.zeros((128, 32), np.float32)
    rbr[0] = f(inputs["router_b"][0])
    shared["router_brow"] = rbr
    bu = f(inputs["exp_b_up"][0])
    shared["b_upT"] = np.ascontiguousarray(bu.reshape(32, 16, 128).transpose(2, 0, 1))
    shared["fg_bc"] = np.ascontiguousarray(np.broadcast_to(f(inputs["final_g"])[None, :], (128, D)))
    shared.update(host_consts())
    maps = []
    for i in range(NCORES):
        m = dict(shared)
        m["x"] = np.ascontiguousarray(x[i * NB:(i + 1) * NB])
        cl = c[i * NB:(i + 1) * NB]
        m["cT"] = np.ascontiguousarray(cl.reshape(NB, KC, 128).transpose(2, 1, 0))
        maps.append(m)
    return maps


def kernel(**inputs):
    kb, _ = build(stage=99)
    maps = prep_inputs(inputs)
    res = run_bass_kernel_spmd(kb.nc, maps, core_ids=list(range(NCORES)))
    return np.concatenate([r["out"] for r in res.results], axis=0).astype(np.float32)
```
